# Optimizing a Trainium2 kernel written in Bass

```python
import math
import jax
import jax.numpy as jnp
from jax import lax
import numpy as np

D_MODEL = 2048
BATCH = 2
SEQ = 8192
DEPTH = 2

GRID_W = 64
CTX_LEN = 256
EPS = 1e-6
CONV_W = 4
CONV_LEFT = 2

D_LRU = D_MODEL
LRU_BLOCKS = 16
LRU_BS = D_LRU // LRU_BLOCKS
LRU_C = 8.0

D_RWKV = D_MODEL
RWKV_HEAD = 64
RWKV_HEADS = D_RWKV // RWKV_HEAD
W_LORA = 96
A_LORA = 96
G_LORA = 256
RWKV_GN_EPS = 64e-5

D_SSM = D_MODEL
SSM_HEADDIM = 64
SSM_HEADS = D_SSM // SSM_HEADDIM
SSM_STATE = 128
SSM_GROUPS = 4
SSM_CHUNK = 128
D_XBC = D_SSM + 2 * SSM_GROUPS * SSM_STATE

D_FF = 3 * D_MODEL
N_EXPERTS = 8
TOP_K = 2
D_FF_EXPERT = D_FF // 2
N_DENSE = (DEPTH + 1) // 2
N_MOE = DEPTH // 2

N_GATE = 3 * D_MODEL
N_LRU_IN = 2 * D_LRU
N_RWKV_IN = 3 * D_RWKV + 2 * W_LORA + 2 * A_LORA + G_LORA
N_SSM_IN = D_SSM + D_XBC + 2 * SSM_HEADS
OFF_LRU = N_GATE
OFF_RWKV = OFF_LRU + N_LRU_IN
OFF_SSM = OFF_RWKV + N_RWKV_IN
N_IN = OFF_SSM + N_SSM_IN

kernel_name = "hybrid_lru_rwkv7_mamba2_moe_dit"


def rms_norm(x, gain):
    xf = x.astype(jnp.float32)
    xf = xf * lax.rsqrt(jnp.mean(xf * xf, axis=-1, keepdims=True) + EPS)
    return (xf * gain).astype(x.dtype)


def centred_dwconv(u, w, b):
    l = u.shape[1]
    up = jnp.pad(u, ((0, 0), (CONV_LEFT, CONV_W - 1 - CONV_LEFT), (0, 0)))
    out = b
    for tap in range(CONV_W):
        out = out + up[:, tap:tap + l] * w[tap]
    return out


def bidir_token_shift(u, mu):
    prev = jnp.pad(u[:, :-1], ((0, 0), (1, 0), (0, 0)))
    nxt = jnp.pad(u[:, 1:], ((0, 0), (0, 1), (0, 0)))
    return u + mu * (0.5 * (prev + nxt) - u)


def grid_transpose(u, rows, cols):
    b, l, ch = u.shape
    return u.reshape(b, rows, cols, ch).transpose(0, 2, 1, 3).reshape(b, l, ch)


def linear_scan(a, bx, h0):
    def combine(lhs, rhs):
        return lhs[0] * rhs[0], rhs[0] * lhs[1] + rhs[1]
    a_cum, h = lax.associative_scan(combine, (a, bx), axis=1)
    return h + a_cum * h0[:, None]


def rglru_direction(xc, gate_w, gate_b, lam, h0, reverse):
    if reverse:
        xc = jnp.flip(xc, axis=1)
    b, l, _ = xc.shape
    xb = xc.reshape(b, l, LRU_BLOCKS, LRU_BS)
    gates = jnp.einsum('blnk,gnkj->gblnj', xb, gate_w).reshape(2, b, l, D_LRU) + gate_b[:, None, None]
    gates = jax.nn.sigmoid(gates.astype(jnp.float32))
    log_a = -LRU_C * gates[0] * jax.nn.softplus(-lam.astype(jnp.float32))
    a = jnp.exp(log_a)
    bx = jnp.sqrt(-jnp.expm1(2.0 * log_a)) * gates[1] * xc
    h = linear_scan(a, bx, h0)
    h_last = h[:, -1]
    if reverse:
        h = jnp.flip(h, axis=1)
    return h, h_last


def rglru_mixer(p_ctx, p_lat, conv_w, conv_b, gate_w, gate_b, lam):
    xc_ctx = centred_dwconv(p_ctx[..., :D_LRU], conv_w, conv_b)
    xc_lat = centred_dwconv(p_lat[..., :D_LRU], conv_w, conv_b)
    h0 = jnp.zeros((p_ctx.shape[0], D_LRU), jnp.float32)
    y_ctx, y_lat = 0.0, 0.0
    for d in range(2):
        h_c, h_c_last = rglru_direction(xc_ctx, gate_w[d], gate_b[d], lam[d], h0, d == 1)
        h_l, _ = rglru_direction(xc_lat, gate_w[d], gate_b[d], lam[d], h_c_last, d == 1)
        y_ctx = y_ctx + h_c
        y_lat = y_lat + h_l
    return (y_ctx * jax.nn.gelu(p_ctx[..., D_LRU:]), y_lat * jax.nn.gelu(p_lat[..., D_LRU:]))


def rwkv7_features(p, mu, w0, w_up, a0, a_up, g_up, k_k, k_a):
    b, l, _ = p.shape
    p = bidir_token_shift(p, mu)
    heads = lambda t: t.reshape(b, l, RWKV_HEADS, RWKV_HEAD)
    r = p[..., 0:D_RWKV]
    k = p[..., D_RWKV:2 * D_RWKV]
    v = p[..., 2 * D_RWKV:3 * D_RWKV]
    o = 3 * D_RWKV
    w_lo = p[..., o:o + 2 * W_LORA].reshape(b, l, 2, W_LORA)
    o = o + 2 * W_LORA
    a_lo = p[..., o:o + 2 * A_LORA].reshape(b, l, 2, A_LORA)
    o = o + 2 * A_LORA
    g = jax.nn.sigmoid(p[..., o:o + G_LORA]) @ g_up
    kk = heads(k * k_k).astype(jnp.float32)
    kk = kk * lax.rsqrt(jnp.sum(kk * kk, axis=-1, keepdims=True) + 1e-12)
    decays, keys, b_vecs = [], [], []
    for d in range(2):
        w = -jax.nn.softplus(-(w0[d] + jnp.tanh(w_lo[:, :, d]) @ w_up[d]).astype(jnp.float32)) - 0.5
        decays.append(heads(jnp.exp(-jnp.exp(w))))
        a = jax.nn.sigmoid(a0[d] + a_lo[:, :, d] @ a_up[d])
        keys.append(heads(k * (1.0 + (a - 1.0) * k_a)))
        b_vecs.append(kk * heads(a))
    return heads(r), heads(v), kk, g, decays, keys, b_vecs


def rwkv7_scan(r, decay, k, v, a_vec, b_vec, s0, reverse):
    xs = tuple(jnp.moveaxis(t, 1, 0) for t in (r, decay, k, v, a_vec, b_vec))

    def step(s, inp):
        r_t, w_t, k_t, v_t, a_t, b_t = inp
        sa = jnp.einsum('bhvk,bhk->bhv', s, a_t)
        s = s * w_t[:, :, None, :] + sa[..., None] * b_t[:, :, None, :] + v_t[..., None] * k_t[:, :, None, :]
        return s, jnp.einsum('bhvk,bhk->bhv', s, r_t)

    s_fin, y = lax.scan(step, s0, xs, reverse=reverse)
    return jnp.moveaxis(y, 0, 1), s_fin


def rwkv7_output(r, v, g, keys, y, r_k, ln_w, ln_b):
    b, l = r.shape[:2]
    yf = y.astype(jnp.float32)
    mean = jnp.mean(yf, axis=-1, keepdims=True)
    var = jnp.mean(jnp.square(yf - mean), axis=-1, keepdims=True)
    yn = ((yf - mean) * lax.rsqrt(var + RWKV_GN_EPS)).reshape(b, l, D_RWKV) * ln_w + ln_b
    bonus = jnp.sum(r * (keys[0] + keys[1]) * r_k, axis=-1, keepdims=True) * v
    return (yn + bonus.reshape(b, l, D_RWKV)) * g


def rwkv7_mixer(p_ctx, p_lat, mu, w0, w_up, a0, a_up, g_up, k_k, k_a, r_k, ln_w, ln_b):
    rc, vc, kkc, gc, dec_c, key_c, bv_c = rwkv7_features(p_ctx, mu, w0, w_up, a0, a_up, g_up, k_k, k_a)
    rl, vl, kkl, gl, dec_l, key_l, bv_l = rwkv7_features(p_lat, mu, w0, w_up, a0, a_up, g_up, k_k, k_a)
    s0 = jnp.zeros((p_ctx.shape[0], RWKV_HEADS, RWKV_HEAD, RWKV_HEAD), jnp.float32)
    y_ctx, y_lat = 0.0, 0.0
    for d in range(2):
        yc, s_c = rwkv7_scan(rc, dec_c[d], key_c[d], vc, -kkc, bv_c[d], s0, d == 1)
        yl, _ = rwkv7_scan(rl, dec_l[d], key_l[d], vl, -kkl, bv_l[d], s_c, d == 1)
        y_ctx = y_ctx + yc
        y_lat = y_lat + yl
    return (rwkv7_output(rc, vc, gc, key_c, y_ctx, r_k, ln_w, ln_b),
            rwkv7_output(rl, vl, gl, key_l, y_lat, r_k, ln_w, ln_b))


def ssd_chunked(x, dt, a, bm, cm, h0):
    b, l, nh, pd = x.shape
    nc, q, g, hg, n = l // SSM_CHUNK, SSM_CHUNK, SSM_GROUPS, SSM_HEADS // SSM_GROUPS, SSM_STATE
    xq = x.reshape(b, nc, q, g, hg, pd)
    dtq = dt.reshape(b, nc, q, g, hg)
    bq = bm.reshape(b, nc, q, g, n)
    cq = cm.reshape(b, nc, q, g, n)
    cum = jnp.cumsum(dtq * a.reshape(g, hg), axis=2)
    seg = cum[:, :, :, None] - cum[:, :, None, :]
    lower = jnp.tril(jnp.ones((q, q), bool))[None, None, :, :, None, None]
    decay_ij = jnp.exp(jnp.where(lower, seg, -jnp.inf))
    cb = jnp.einsum('bcign,bcjgn->bcijg', cq, bq)
    scores = cb[..., None] * decay_ij * dtq[:, :, None]
    y_diag = jnp.einsum('bcijgh,bcjghp->bcighp', scores, xq)
    to_end = jnp.exp(cum[:, :, -1:] - cum) * dtq
    states = jnp.einsum('bcjgn,bcjgh,bcjghp->bcghpn', bq, to_end, xq)
    chunk_decay = jnp.exp(cum[:, :, -1])

    def step(h, inp):
        st, dc = inp
        return h * dc[..., None, None] + st, h

    h_fin, h_enter = lax.scan(step, h0.reshape(b, g, hg, pd, n),
                              (jnp.moveaxis(states, 1, 0), jnp.moveaxis(chunk_decay, 1, 0)))
    h_enter = jnp.moveaxis(h_enter, 0, 1)
    y_off = jnp.einsum('bcign,bcghpn->bcighp', cq, h_enter) * jnp.exp(cum)[..., None]
    return (y_diag + y_off).reshape(b, l, nh, pd), h_fin.reshape(b, nh, pd, n)


def ssd_direction(x, dt, a, bm, cm, h0, reverse):
    if reverse:
        x, dt, bm, cm = (jnp.flip(t, axis=1) for t in (x, dt, bm, cm))
    y, h_fin = ssd_chunked(x, dt, a, bm, cm, h0)
    if reverse:
        y = jnp.flip(y, axis=1)
    return y, h_fin


def mamba2_prepare(p, conv_w, conv_b):
    b, l, _ = p.shape
    z = p[..., :D_SSM]
    xbc = jax.nn.silu(centred_dwconv(p[..., D_SSM:D_SSM + D_XBC], conv_w, conv_b))
    xs = xbc[..., :D_SSM].reshape(b, l, SSM_HEADS, SSM_HEADDIM)
    bm = xbc[..., D_SSM:D_SSM + SSM_GROUPS * SSM_STATE].reshape(b, l, SSM_GROUPS, SSM_STATE)
    cm = xbc[..., D_SSM + SSM_GROUPS * SSM_STATE:].reshape(b, l, SSM_GROUPS, SSM_STATE)
    dt_raw = p[..., D_SSM + D_XBC:].reshape(b, l, 2, SSM_HEADS).astype(jnp.float32)
    return z, xs, bm, cm, dt_raw


def gated_rms_norm(y, z, gain):
    b, l, _ = z.shape
    yg = (y.reshape(b, l, D_SSM) * jax.nn.silu(z.astype(jnp.float32))).reshape(b, l, SSM_GROUPS, D_SSM // SSM_GROUPS)
    yg = yg * lax.rsqrt(jnp.mean(yg * yg, axis=-1, keepdims=True) + EPS)
    return yg.reshape(b, l, D_SSM) * gain


def mamba2_mixer(p_ctx, p_lat, conv_w, conv_b, a_log, dt_bias, d_skip, norm_w):
    zc, xc, bc, cc, dtc = mamba2_prepare(p_ctx, conv_w, conv_b)
    zl, xl, bl, cl, dtl = mamba2_prepare(p_lat, conv_w, conv_b)
    a = -jnp.exp(a_log.astype(jnp.float32))
    h0 = jnp.zeros((p_ctx.shape[0], SSM_HEADS, SSM_HEADDIM, SSM_STATE), jnp.float32)
    y_ctx = d_skip[:, None] * xc
    y_lat = d_skip[:, None] * xl
    for d in range(2):
        dt_c = jax.nn.softplus(dtc[:, :, d] + dt_bias[d])
        dt_l = jax.nn.softplus(dtl[:, :, d] + dt_bias[d])
        yc, h_c = ssd_direction(xc, dt_c, a[d], bc, cc, h0, d == 1)
        yl, _ = ssd_direction(xl, dt_l, a[d], bl, cl, h_c, d == 1)
        y_ctx = y_ctx + yc
        y_lat = y_lat + yl
    return gated_rms_norm(y_ctx, zc, norm_w), gated_rms_norm(y_lat, zl, norm_w)


def merge_branches(p_gate, y_lru, y_rwkv, y_ssm, w_out_lru, w_out_rwkv, w_out_ssm, w_o):
    g = jax.nn.sigmoid(p_gate)
    m = (g[..., :D_MODEL] * (y_lru @ w_out_lru)
         + g[..., D_MODEL:2 * D_MODEL] * (y_rwkv @ w_out_rwkv)
         + g[..., 2 * D_MODEL:] * (y_ssm @ w_out_ssm))
    return m @ w_o


def swiglu(u, w1, w3, w2):
    return (jax.nn.silu(u @ w1) * (u @ w3)) @ w2


def moe_swiglu(u, router, w1, w3, w2):
    shp = u.shape
    t = u.reshape(-1, shp[-1])
    logits = (t @ router).astype(jnp.float32)
    top_v, top_i = lax.top_k(logits, TOP_K)
    probs = jax.nn.softmax(top_v, axis=-1)
    gates = jnp.sum(jax.nn.one_hot(top_i, N_EXPERTS, dtype=probs.dtype) * probs[..., None], axis=-2)
    y = 0.0
    for e in range(N_EXPERTS):
        y = y + gates[:, e:e + 1] * swiglu(t, w1[e], w3[e], w2[e])
    return y.reshape(shp)


def channel_mixer(u, li, ffn_w1, ffn_w3, ffn_w2, moe_router, moe_w1, moe_w3, moe_w2):
    j = li // 2
    if li % 2 == 0:
        return swiglu(u, ffn_w1[j], ffn_w3[j], ffn_w2[j])
    return moe_swiglu(u, moe_router[j], moe_w1[j], moe_w3[j], moe_w2[j])


def setup_inputs(seed: int = 0) -> dict:
    key = jax.random.key(seed)
    ks = iter(jax.random.split(key, 64))
    f32 = jnp.float32

    def nrm(shape, scale):
        return jax.random.normal(next(ks), shape, f32) * scale

    def unif(shape, lo, hi):
        return jax.random.uniform(next(ks), shape, f32, lo, hi)

    lam_a = unif((DEPTH, 2, D_LRU), 0.9, 0.999)
    dt0 = jnp.exp(unif((DEPTH, 2, SSM_HEADS), math.log(1e-3), math.log(1e-1)))
    return {
        "x": nrm((BATCH, SEQ, D_MODEL), 1.0),
        "c": nrm((BATCH, D_MODEL), 1.0),
        "ctx": nrm((BATCH, CTX_LEN, D_MODEL), 1.0),
        "c_ctx": nrm((D_MODEL,), 1.0),
        "ada_w": nrm((DEPTH, D_MODEL, 6 * D_MODEL), 0.5 * D_MODEL ** -0.5),
        "ada_b": nrm((DEPTH, 6 * D_MODEL), 0.02),
        "norm_mix": 1.0 + nrm((DEPTH, D_MODEL), 0.02),
        "norm_ffn": 1.0 + nrm((DEPTH, D_MODEL), 0.02),
        "norm_final": 1.0 + nrm((D_MODEL,), 0.02),
        "w_in": nrm((DEPTH, D_MODEL, N_IN), D_MODEL ** -0.5),
        "lru_conv_w": nrm((DEPTH, CONV_W, D_LRU), 0.5),
        "lru_conv_b": nrm((DEPTH, D_LRU), 0.02),
        "lru_gate_w": nrm((DEPTH, 2, 2, LRU_BLOCKS, LRU_BS, LRU_BS), LRU_BS ** -0.5),
        "lru_gate_b": nrm((DEPTH, 2, 2, D_LRU), 0.1),
        "lru_lambda": jnp.log(lam_a) - jnp.log1p(-lam_a),
        "rwkv_mu": unif((DEPTH, N_RWKV_IN), 0.0, 1.0),
        "rwkv_w0": unif((DEPTH, 2, D_RWKV), -6.0, -1.0),
        "rwkv_w_up": nrm((DEPTH, 2, W_LORA, D_RWKV), 0.5 * W_LORA ** -0.5),
        "rwkv_a0": nrm((DEPTH, 2, D_RWKV), 0.3),
        "rwkv_a_up": nrm((DEPTH, 2, A_LORA, D_RWKV), 0.5 * A_LORA ** -0.5),
        "rwkv_g_up": nrm((DEPTH, G_LORA, D_RWKV), G_LORA ** -0.5),
        "rwkv_k_k": 0.85 + nrm((DEPTH, D_RWKV), 0.05),
        "rwkv_k_a": 1.0 + nrm((DEPTH, D_RWKV), 0.05),
        "rwkv_r_k": nrm((DEPTH, RWKV_HEADS, RWKV_HEAD), 0.1),
        "rwkv_ln_w": 1.0 + nrm((DEPTH, D_RWKV), 0.02),
        "rwkv_ln_b": nrm((DEPTH, D_RWKV), 0.02),
        "ssm_conv_w": nrm((DEPTH, CONV_W, D_XBC), 0.5),
        "ssm_conv_b": nrm((DEPTH, D_XBC), 0.02),
        "ssm_a_log": jnp.log(unif((DEPTH, 2, SSM_HEADS), 1.0, 16.0)),
        "ssm_dt_bias": dt0 + jnp.log(-jnp.expm1(-dt0)),
        "ssm_d": 1.0 + nrm((DEPTH, SSM_HEADS), 0.1),
        "ssm_norm_w": 1.0 + nrm((DEPTH, D_SSM), 0.02),
        "w_out_lru": nrm((DEPTH, D_LRU, D_MODEL), D_LRU ** -0.5),
        "w_out_rwkv": nrm((DEPTH, D_RWKV, D_MODEL), D_RWKV ** -0.5),
        "w_out_ssm": nrm((DEPTH, D_SSM, D_MODEL), D_SSM ** -0.5),
        "w_o": nrm((DEPTH, D_MODEL, D_MODEL), D_MODEL ** -0.5),
        "ffn_w1": nrm((N_DENSE, D_MODEL, D_FF), D_MODEL ** -0.5),
        "ffn_w3": nrm((N_DENSE, D_MODEL, D_FF), D_MODEL ** -0.5),
        "ffn_w2": nrm((N_DENSE, D_FF, D_MODEL), D_FF ** -0.5),
        "moe_router": nrm((N_MOE, D_MODEL, N_EXPERTS), D_MODEL ** -0.5),
        "moe_w1": nrm((N_MOE, N_EXPERTS, D_MODEL, D_FF_EXPERT), D_MODEL ** -0.5),
        "moe_w3": nrm((N_MOE, N_EXPERTS, D_MODEL, D_FF_EXPERT), D_MODEL ** -0.5),
        "moe_w2": nrm((N_MOE, N_EXPERTS, D_FF_EXPERT, D_MODEL), D_FF_EXPERT ** -0.5),
    }


def reference(x, c, ctx, c_ctx, ada_w, ada_b, norm_mix, norm_ffn, norm_final, w_in,
              lru_conv_w, lru_conv_b, lru_gate_w, lru_gate_b, lru_lambda,
              rwkv_mu, rwkv_w0, rwkv_w_up, rwkv_a0, rwkv_a_up, rwkv_g_up, rwkv_k_k, rwkv_k_a,
              rwkv_r_k, rwkv_ln_w, rwkv_ln_b,
              ssm_conv_w, ssm_conv_b, ssm_a_log, ssm_dt_bias, ssm_d, ssm_norm_w,
              w_out_lru, w_out_rwkv, w_out_ssm, w_o,
              ffn_w1, ffn_w3, ffn_w2, moe_router, moe_w1, moe_w3, moe_w2):
    rows = x.shape[1] // GRID_W
    h_lat, h_ctx = x, ctx
    for li in range(DEPTH):
        last = li == DEPTH - 1
        mod_lat = (jax.nn.silu(c) @ ada_w[li] + ada_b[li])[:, None, :]
        mod_ctx = (jax.nn.silu(c_ctx) @ ada_w[li] + ada_b[li])[None, None, :]
        shm_l, scm_l, gm_l, shf_l, scf_l, gf_l = jnp.split(mod_lat, 6, axis=-1)
        shm_c, scm_c, gm_c, shf_c, scf_c, gf_c = jnp.split(mod_ctx, 6, axis=-1)

        u_lat = rms_norm(h_lat, norm_mix[li]) * (1.0 + scm_l) + shm_l
        u_ctx = rms_norm(h_ctx, norm_mix[li]) * (1.0 + scm_c) + shm_c
        if li % 2 == 1:
            u_lat = grid_transpose(u_lat, rows, GRID_W)
        p_lat = u_lat @ w_in[li]
        p_ctx = u_ctx @ w_in[li]
        ya_c, ya_l = rglru_mixer(p_ctx[..., OFF_LRU:OFF_RWKV], p_lat[..., OFF_LRU:OFF_RWKV],
                                 lru_conv_w[li], lru_conv_b[li], lru_gate_w[li], lru_gate_b[li],
                                 lru_lambda[li])
        yb_c, yb_l = rwkv7_mixer(p_ctx[..., OFF_RWKV:OFF_SSM], p_lat[..., OFF_RWKV:OFF_SSM],
                                 rwkv_mu[li], rwkv_w0[li], rwkv_w_up[li], rwkv_a0[li], rwkv_a_up[li],
                                 rwkv_g_up[li], rwkv_k_k[li], rwkv_k_a[li], rwkv_r_k[li],
                                 rwkv_ln_w[li], rwkv_ln_b[li])
        yc_c, yc_l = mamba2_mixer(p_ctx[..., OFF_SSM:], p_lat[..., OFF_SSM:],
                                  ssm_conv_w[li], ssm_conv_b[li], ssm_a_log[li], ssm_dt_bias[li],
                                  ssm_d[li], ssm_norm_w[li])
        o_lat = merge_branches(p_lat[..., :OFF_LRU], ya_l, yb_l, yc_l,
                               w_out_lru[li], w_out_rwkv[li], w_out_ssm[li], w_o[li])
        if li % 2 == 1:
            o_lat = grid_transpose(o_lat, GRID_W, rows)
        h_lat = h_lat + gm_l * o_lat
        if not last:
            o_ctx = merge_branches(p_ctx[..., :OFF_LRU], ya_c, yb_c, yc_c,
                                   w_out_lru[li], w_out_rwkv[li], w_out_ssm[li], w_o[li])
            h_ctx = h_ctx + gm_c * o_ctx

        v_lat = rms_norm(h_lat, norm_ffn[li]) * (1.0 + scf_l) + shf_l
        h_lat = h_lat + gf_l * channel_mixer(v_lat, li, ffn_w1, ffn_w3, ffn_w2,
                                             moe_router, moe_w1, moe_w3, moe_w2)
        if not last:
            v_ctx = rms_norm(h_ctx, norm_ffn[li]) * (1.0 + scf_c) + shf_c
            h_ctx = h_ctx + gf_c * channel_mixer(v_ctx, li, ffn_w1, ffn_w3, ffn_w2,
                                                 moe_router, moe_w1, moe_w3, moe_w2)
    return rms_norm(h_lat, norm_final)
```

```python
import contextlib
import numpy as np
import concourse.bass as bass
import concourse.mybir as mybir

F32 = mybir.dt.float32
BF16 = mybir.dt.bfloat16
AF = mybir.ActivationFunctionType
ALU = mybir.AluOpType
AX = mybir.AxisListType


class Tl:
    __slots__ = ("t", "w", "r", "name")

    def __init__(self, t, name):
        self.t = t
        self.w = None
        self.r = {}
        self.name = name

    def __getitem__(self, idx):
        return self.t[idx]


class Sub:
    def __init__(self, parent, ap, name):
        self.parent = parent
        self.t = ap
        self.name = name

    def __getitem__(self, idx):
        return self.t[idx]

    @property
    def w(self):
        return self.parent.w

    @w.setter
    def w(self, v):
        self.parent.w = v

    @property
    def r(self):
        return self.parent.r

    @r.setter
    def r(self, v):
        self.parent.r = v


class KB:
    NRING = 24

    def __init__(self, nc):
        self.nc = nc
        self.E = {"pe": nc.tensor, "act": nc.scalar, "dve": nc.vector, "pool": nc.gpsimd, "sp": nc.sync}
        self.es = contextlib.ExitStack()
        self.sem = {}
        self.cnt = {}
        for e in ("pe", "act", "dve", "pool"):
            self.sem[e] = self.es.enter_context(nc.semaphore("s_" + e))
            self.cnt[e] = 0
        for i in range(self.NRING):
            self.sem[("d", i)] = self.es.enter_context(nc.semaphore("s_d%d" % i))
            self.cnt[("d", i)] = 0
        self.ring = 0
        self.seen = {e: {} for e in self.E}
        self.ps_tiles = []
        self.ps_i = 0
        self.uid = 0
        self.nwait = 0

    def sb(self, shape, dt=F32, name=None, stack=None):
        self.uid += 1
        name = (name or "t") + "_%d" % self.uid
        t = (stack or self.es).enter_context(self.nc.sbuf_tensor(name, list(shape), dt))
        return Tl(t, name)

    def init_psum(self, n=8, cols=512):
        for i in range(n):
            t = self.es.enter_context(self.nc.psum_tensor("ps%d" % i, [128, cols], F32))
            self.ps_tiles.append(Tl(t, "ps%d" % i))

    def ps(self):
        t = self.ps_tiles[self.ps_i % len(self.ps_tiles)]
        self.ps_i += 1
        return t

    def dram(self, name, shape, dt=F32, kind="Internal"):
        return self.nc.dram_tensor(name, list(shape), dt, kind=kind).ap()

    def _need(self, eng, key, val, war=False):
        if key == eng and eng == "pe":
            return
        if self.seen[eng].get(key, 0) >= val:
            return
        self.E[eng].wait_ge(self.sem[key], val)
        self.nwait += 1
        self.seen[eng][key] = val

    def _sync(self, eng, r, w):
        for t in r:
            if t.w is not None:
                self._need(eng, *t.w)
        for t in w:
            if t.w is not None:
                self._need(eng, *t.w)
            for key, val in t.r.items():
                self._need(eng, key, val, war=True)

    def _post(self, ev, r, w):
        for t in w:
            t.w = ev
            t.r = {}
        for t in r:
            if t.r.get(ev[0], 0) < ev[1]:
                t.r[ev[0]] = ev[1]

    def I(self, eng, meth, r, w, *args, **kw):
        self._sync(eng, r, w)
        ins = getattr(self.E[eng], meth)(*args, **kw)
        self.cnt[eng] += 1
        ins.then_inc(self.sem[eng], 1)
        self._post((eng, self.cnt[eng]), r, w)
        return ins

    def dma(self, out, in_, r=(), w=(), q="sp", **kw):
        if q == "pool":
            q = "sp"
        key = ("d", self.ring % self.NRING)
        self.ring += 1
        self._sync(q, r, w)
        self._need(q, key, self.cnt[key])
        ins = self.E[q].dma_start(out=out, in_=in_, **kw)
        self.cnt[key] += 16
        ins.then_inc(self.sem[key], 16)
        self._post((key, self.cnt[key]), r, w)

    def barrier(self, engines=("pe", "act", "dve", "pool", "sp")):
        for e in engines:
            for key, c in self.cnt.items():
                if c > 0:
                    self._need(e, key, c)

    def finish(self):
        self.barrier()
        self.es.close()

    def mm(self, ps_ap, lhsT, rhs, r, w, start=True, stop=True):
        return self.I("pe", "matmul", r, w, ps_ap, lhsT=lhsT, rhs=rhs, start=start, stop=stop)

    def tr(self, ps_ap, in_ap, ident_ap, r, w):
        return self.I("pe", "transpose", r, w, ps_ap, in_ap, ident_ap)

    def act(self, out, in_, func, r, w, **kw):
        return self.I("act", "activation", r, w, out=out, in_=in_, func=func, **kw)

    def tt(self, out, in0, in1, op, r, w, eng="dve"):
        return self.I(eng, "tensor_tensor", r, w, out=out, in0=in0, in1=in1, op=op)

    def ts(self, out, in0, s1, s2, op0, op1, r, w, eng="dve"):
        if op1 is None:
            return self.I(eng, "tensor_scalar", r, w, out=out, in0=in0, scalar1=s1, scalar2=None, op0=op0)
        return self.I(eng, "tensor_scalar", r, w, out=out, in0=in0, scalar1=s1, scalar2=s2, op0=op0, op1=op1)

    def stt(self, out, in0, scalar, in1, op0, op1, r, w):
        return self.I("dve", "scalar_tensor_tensor", r, w, out=out, in0=in0, scalar=scalar, in1=in1, op0=op0, op1=op1)

    def copy(self, out, in_, r, w, eng="dve"):
        if eng == "act":
            return self.I("act", "copy", r, w, out=out, in_=in_)
        return self.I(eng, "tensor_copy", r, w, out=out, in_=in_)

    def memset(self, t, val, eng="pool"):
        return self.I(eng, "memset", [], [t], t[:], val)


D = 2048
KC = 16


class WS:
    def __init__(self, k, nstg=4, nbf=3, look=3, cast_engs=("dve", "pool", "act")):
        self.k = k
        self.stg = [k.sb([128, KC, 128], F32, "wstg") for _ in range(nstg)]
        self.bf = [k.sb([128, KC, 128], BF16, "wbf") for _ in range(nbf)]
        self.look = look
        self.cast_engs = cast_engs
        self.pieces = []
        self.n_dma = 0
        self.n_cast = 0

    def set(self, pieces):
        self.pieces = pieces
        self.n_dma = 0
        self.n_cast = 0

    def _dma(self):
        i = self.n_dma
        if i >= len(self.pieces):
            return
        view, kc, m = self.pieces[i]
        st = self.stg[i % len(self.stg)]
        q = "sp" if i % 2 == 0 else "pool"
        self.k.dma(st[:, 0:kc, 0:m], view, w=[st], q=q)
        self.n_dma += 1

    def _cast(self):
        i = self.n_cast
        if i >= len(self.pieces):
            return
        view, kc, m = self.pieces[i]
        st = self.stg[i % len(self.stg)]
        bf = self.bf[i % len(self.bf)]
        eng = self.cast_engs[i % len(self.cast_engs)]
        self.k.copy(bf[:, 0:kc, 0:m], st[:, 0:kc, 0:m], [st], [bf], eng=eng)
        self.n_cast += 1

    def take(self, i):
        while self.n_dma < min(len(self.pieces), i + 1 + self.look):
            self._dma()
        while self.n_cast < min(len(self.pieces), i + 2):
            self._cast()
        return self.bf[i % len(self.bf)]


def wview(w, k0, kc, c0, m):
    return w[k0 * 128:(k0 + kc) * 128, c0:c0 + m].rearrange("(c p) n -> p c n", p=128)


def run_jobs(k, ws, jobs):
    pieces = []
    for jb in jobs:
        jb["p0"] = len(pieces)
        pieces.extend(jb["pieces"])
    ws.set(pieces)
    for jb in jobs:
        if jb.get("pre"):
            jb["pre"]()
        ps = k.ps()
        x = jb["x"]
        nt = jb["nt"]
        np_ = len(jb["pieces"])
        kbase = 0
        for pi, (view, kc, m) in enumerate(jb["pieces"]):
            bf = ws.take(jb["p0"] + pi)
            for c in range(kc):
                k.mm(ps[0:m, 0:nt], bf[:, c, 0:m], x[:, kbase + c, 0:nt], [bf, x], [ps],
                     start=(pi == 0 and c == 0), stop=(pi == np_ - 1 and c == kc - 1))
            kbase += kc
        jb["epi"](ps)


def modulation(k, cT, ada_w, ada_b_l, ncols_chunks, col_chunk0, ident=None):
    mod = k.sb([128, ncols_chunks, 2], F32, "mod")
    with contextlib.ExitStack() as st:
        c_t = k.sb([128, KC, 2], F32, "c_t", st)
        sc_t = k.sb([128, KC, 2], F32, "sc_t", st)
        k.dma(c_t[:], cT.rearrange("(c p) j -> p c j", p=128), w=[c_t])
        k.act(sc_t[:], c_t[:], AF.Silu, [c_t], [sc_t])
        bl = k.sb([128, ncols_chunks], F32, "adab", st)
        k.dma(bl[:], ada_b_l[:, col_chunk0:col_chunk0 + ncols_chunks], w=[bl])
        wbuf = [k.sb([128, KC, 512], F32, "adaw", st) for _ in range(2)]
        ngrp = (ncols_chunks + 3) // 4
        for g in range(ngrp):
            wb = wbuf[g % 2]
            ncc = min(4, ncols_chunks - g * 4)
            c0 = (col_chunk0 + g * 4) * 128
            k.dma(wb[:, :, 0:ncc * 128], wview(ada_w, 0, KC, c0, ncc * 128), w=[wb], q="sp" if g % 2 == 0 else "pool")
            for cc in range(ncc):
                ps = k.ps()
                for c in range(KC):
                    k.mm(ps[:, 0:2], wb[:, c, cc * 128:(cc + 1) * 128], sc_t[:, c, :], [wb, sc_t], [ps],
                         start=(c == 0), stop=(c == KC - 1))
                k.tt(mod[:, g * 4 + cc, :], ps[:, 0:2], bl[:, g * 4 + cc:g * 4 + cc + 1].to_broadcast([128, 2]), ALU.add,
                     [ps, bl], [mod])
        k.barrier(("sp", "pool", "pe", "dve", "act"))
    return mod


def norm_mod(k, H, nt, ones, A, sh, j, out_bf, tmp32, ssum, rstd, out32=None, sh_off=0):
    k.tt(tmp32[:, :, 0:nt], H[:, :, 0:nt], H[:, :, 0:nt], ALU.mult, [H], [tmp32])
    k.I("dve", "tensor_reduce", [tmp32], [ssum], out=ssum[:, 0:nt], in_=tmp32[:, :, 0:nt].rearrange("p c t -> p t c"),
        axis=AX.X, op=ALU.add)
    ps = k.ps()
    k.mm(ps[:, 0:nt], ones[:, :], ssum[:, 0:nt], [ones, ssum], [ps])
    k.act(rstd[:, 0:nt], ps[:, 0:nt], AF.Sqrt, [ps], [rstd], scale=1.0 / D, bias=k.eps_t[:, 0:1])
    k.I("dve", "reciprocal", [rstd], [rstd], out=rstd[:, 0:nt], in_=rstd[:, 0:nt])
    k.tt(tmp32[:, :, 0:nt], H[:, :, 0:nt], rstd[:, 0:nt].unsqueeze(1).to_broadcast([128, KC, nt]), ALU.mult,
         [H, rstd], [tmp32])
    for c in range(KC):
        for o in (out_bf, out32):
            if o is None:
                continue
            if sh is not None:
                k.ts(o[:, c, 0:nt], tmp32[:, c, 0:nt], A[:, c, j:j + 1], sh[:, sh_off + c, j:j + 1], ALU.mult, ALU.add,
                     [tmp32, A, sh], [o])
            else:
                k.ts(o[:, c, 0:nt], tmp32[:, c, 0:nt], A[:, c, j:j + 1], None, ALU.mult, None, [tmp32, A], [o])


def consts(k):
    k.eps_t = k.sb([128, 1], F32, "eps")
    k.memset(k.eps_t, 1e-6)
    ones = k.sb([128, 128], F32, "ones")
    k.memset(ones, 1.0)
    ident = k.sb([128, 128], F32, "ident")
    k.memset(ident, 1.0)
    k.I("pool", "affine_select", [ident], [ident], out=ident[:], in_=ident[:], pattern=[[-1, 128]],
        compare_op=ALU.is_equal, fill=0.0, base=0, channel_multiplier=1)
    return ones, ident


def build_B(NL, NCX, kind, final_norm, NT=256):
    NB = NL + NCX
    nc = bass.Bass("TRN2", target_bir_lowering=False)
    k = KB(nc)
    inp = lambda n, s: k.dram(n, s, F32, "ExternalInput")
    hT = inp("hT", [D, NB]); yT = inp("yT", [3, D, NB]); cT = inp("cT", [D, 2])
    ada_w = inp("ada_w", [D, 6 * D]); ada_b_l = inp("ada_b_l", [128, 96])
    nmix = inp("nmix", [128, KC]); nffn = inp("nffn", [128, KC]); nfin = inp("nfin", [128, KC])
    w_gate = inp("w_gate", [D, 3 * D]); w_outs = inp("w_outs", [3, D, D]); w_o = inp("w_o", [D, D])
    if kind == "dense":
        w1 = inp("w1", [D, 3 * D]); w3 = inp("w3", [D, 3 * D]); w2 = inp("w2", [3 * D, D])
    else:
        router = inp("router", [D, 8]); mw1 = inp("mw1", [8, D, 3072]); mw3 = inp("mw3", [8, D, 3072])
        mw2 = inp("mw2", [8, 3072, D])
    outT = k.dram("outT", [D, NB], F32, "ExternalOutput")

    k.init_psum()
    ones, ident = consts(k)
    mod = modulation(k, cT, ada_w, ada_b_l, 96, 0)
    nm = k.sb([128, KC], F32, "nm"); nf = k.sb([128, KC], F32, "nf"); nfi = k.sb([128, KC, 2], F32, "nfi")
    k.dma(nm[:], nmix, w=[nm]); k.dma(nf[:], nffn, w=[nf])
    nfi1 = k.sb([128, KC], F32, "nfi1"); k.dma(nfi1[:], nfin, w=[nfi1])
    k.copy(nfi[:], nfi1[:].unsqueeze(2).to_broadcast([128, KC, 2]), [nfi1], [nfi])
    A_mix = k.sb([128, KC, 2], F32, "A_mix"); A_ffn = k.sb([128, KC, 2], F32, "A_ffn")
    for (A, g, sec) in ((A_mix, nm, 1), (A_ffn, nf, 4)):
        k.ts(A[:], mod[:, sec * KC:(sec + 1) * KC, :], 1.0, None, ALU.add, None, [mod], [A])
        k.tt(A[:], A[:], g[:].unsqueeze(2).to_broadcast([128, KC, 2]), ALU.mult, [A, g], [A])
    shm = mod
    SHM, GM, SHF, GF = 0, 2, 3, 5

    H = k.sb([128, KC, NT], F32, "H"); U = k.sb([128, KC, NT], BF16, "U"); M = k.sb([128, KC, NT], F32, "M")
    big32 = k.sb([128, KC, NT], F32, "big32"); YB = k.sb([128, KC, NT], BF16, "YB")
    MB = k.sb([128, KC, NT], BF16, "MB")
    nh = 48 if kind == "dense" else 24
    HID = k.sb([128, nh, NT], BF16, "HID")
    ssum = k.sb([128, NT], F32, "ssum"); rstd = k.sb([128, NT], F32, "rstd")
    gs = [k.sb([128, NT], F32, "gs") for _ in range(2)]
    tmpe = [k.sb([128, NT], F32, "tmpe") for _ in range(2)]
    ws = WS(k)
    if kind == "moe":
        rt = k.sb([128, KC, 8], F32, "rt")
        k.dma(rt[:], router.rearrange("(c p) e -> p c e", p=128), w=[rt])
        V32 = big32
        selE = k.sb([8, 8, 128], F32, "selE")
        k.memset(selE, 1.0)
        k.I("pool", "affine_select", [selE], [selE], out=selE[:], in_=selE[:], pattern=[[-1, 8], [0, 128]],
            compare_op=ALU.is_equal, fill=0.0, base=0, channel_multiplier=1)
        lg = k.sb([8, NT], F32, "lg"); nblk = (NT + 127) // 128
        ltok = k.sb([128, nblk, 8], F32, "ltok"); t8 = k.sb([128, nblk, 8], F32, "t8"); t8b = k.sb([128, nblk, 8], F32, "t8b")
        m1 = k.sb([128, nblk], F32, "m1"); m2 = k.sb([128, nblk], F32, "m2")
        gT = k.sb([8, NT], F32, "gT"); gbc = k.sb([128, 8, NT], F32, "gbc")
        ACC = M

    OUT = k.sb([128, KC, NT], F32, "OUT") if final_norm else None
    tiles = [(t0, min(NT, NL - t0), 0) for t0 in range(0, NL, NT)]
    if NCX:
        tiles += [(NL + t0, min(NT, NCX - t0), 1) for t0 in range(0, NCX, NT)]

    def tview(ap2d, t0, nt):
        return ap2d[:, t0:t0 + nt].rearrange("(c p) t -> p c t", p=128)

    cnt = [0]
    for (t0, nt, j) in tiles:
        jobs = []

        def pre_tile(t0=t0, nt=nt, j=j):
            k.dma(H[:, :, 0:nt], tview(hT, t0, nt), w=[H])
            norm_mod(k, H, nt, ones, A_mix, mod, j, U, big32, ssum, rstd, sh_off=SHM * KC)

        first = [True]
        for br in range(3):
            def pre_br(br=br, t0=t0, nt=nt):
                k.dma(big32[:, :, 0:nt], tview(yT[br], t0, nt), w=[big32], q="pool")
                k.copy(YB[:, :, 0:nt], big32[:, :, 0:nt], [big32], [YB])

            for oc in range(KC):
                i = cnt[0]; cnt[0] += 1
                g_t = gs[i % 2]; te = tmpe[i % 2]

                def epi_g(ps, g_t=g_t, nt=nt):
                    k.act(g_t[:, 0:nt], ps[:, 0:nt], AF.Sigmoid, [ps], [g_t])

                def epi_y(ps, g_t=g_t, te=te, br=br, oc=oc, nt=nt):
                    if br == 0:
                        k.tt(M[:, oc, 0:nt], ps[:, 0:nt], g_t[:, 0:nt], ALU.mult, [ps, g_t], [M])
                    else:
                        k.tt(te[:, 0:nt], ps[:, 0:nt], g_t[:, 0:nt], ALU.mult, [ps, g_t], [te])
                        k.tt(M[:, oc, 0:nt], M[:, oc, 0:nt], te[:, 0:nt], ALU.add, [M, te], [M], eng="pool")

                pre = None
                if br == 0 and oc == 0:
                    def pre(pre_tile=pre_tile, pre_br=pre_br):
                        pre_tile(); pre_br()
                elif oc == 0:
                    pre = pre_br
                jobs.append(dict(pre=pre, pieces=[(wview(w_gate, 0, KC, br * D + oc * 128, 128), KC, 128)], x=U, nt=nt, epi=epi_g))
                jobs.append(dict(pieces=[(wview(w_outs[br], 0, KC, oc * 128, 128), KC, 128)], x=YB, nt=nt, epi=epi_y))

        def pre_o(nt=nt):
            k.copy(MB[:, :, 0:nt], M[:, :, 0:nt], [M], [MB])

        for oc in range(KC):
            def epi_o(ps, oc=oc, nt=nt, j=j):
                k.stt(H[:, oc, 0:nt], ps[:, 0:nt], mod[:, GM * KC + oc, j:j + 1], H[:, oc, 0:nt], ALU.mult, ALU.add,
                      [ps, mod, H], [H])
            jobs.append(dict(pre=pre_o if oc == 0 else None, pieces=[(wview(w_o, 0, KC, oc * 128, 128), KC, 128)], x=MB, nt=nt, epi=epi_o))

        def pre_ffn(nt=nt, j=j):
            norm_mod(k, H, nt, ones, A_ffn, mod, j, U, big32, ssum, rstd, sh_off=SHF * KC)

        if kind == "dense":
            for hc in range(48):
                i = cnt[0]; cnt[0] += 1
                sa = gs[i % 2]

                def epi_a(ps, sa=sa, nt=nt):
                    k.act(sa[:, 0:nt], ps[:, 0:nt], AF.Silu, [ps], [sa])

                def epi_b(ps, sa=sa, hc=hc, nt=nt):
                    k.tt(HID[:, hc, 0:nt], ps[:, 0:nt], sa[:, 0:nt], ALU.mult, [ps, sa], [HID])
                jobs.append(dict(pre=pre_ffn if hc == 0 else None, pieces=[(wview(w1, 0, KC, hc * 128, 128), KC, 128)], x=U, nt=nt, epi=epi_a))
                jobs.append(dict(pieces=[(wview(w3, 0, KC, hc * 128, 128), KC, 128)], x=U, nt=nt, epi=epi_b))
            for oc in range(KC):
                def epi_f(ps, oc=oc, nt=nt, j=j):
                    k.stt(H[:, oc, 0:nt], ps[:, 0:nt], mod[:, GF * KC + oc, j:j + 1], H[:, oc, 0:nt], ALU.mult, ALU.add,
                          [ps, mod, H], [H])
                jobs.append(dict(pieces=[(wview(w2, kk * KC, KC, oc * 128, 128), KC, 128) for kk in range(3)], x=HID, nt=nt, epi=epi_f))
        else:
            def pre_moe(nt=nt, j=j, t0=t0):
                norm_mod(k, H, nt, ones, A_ffn, mod, j, U, big32, ssum, rstd, sh_off=SHF * KC)
                for c in range(KC):
                    k.ts(big32[:, c, 0:nt], big32[:, c, 0:nt], A_ffn[:, c, j:j + 1], mod[:, SHF * KC + c, j:j + 1],
                         ALU.mult, ALU.add, [big32, A_ffn, mod], [big32])
                ps = k.ps()
                for c in range(KC):
                    k.mm(ps[0:8, 0:nt], rt[:, c, :], big32[:, c, 0:nt], [rt, big32], [ps], start=(c == 0), stop=(c == KC - 1))
                k.copy(lg[:, 0:nt], ps[0:8, 0:nt], [ps], [lg])
                nb = (nt + 127) // 128
                for b in range(nb):
                    w_ = min(128, nt - b * 128)
                    ps2 = k.ps()
                    k.tr(ps2[0:w_, 0:8], lg[0:8, b * 128:b * 128 + w_], ident[0:8, 0:8], [lg, ident], [ps2])
                    k.copy(ltok[0:w_, b, :], ps2[0:w_, 0:8], [ps2], [ltok])
                if nt % 128:
                    pass
                P = min(128, nt)
                bc = lambda t_: t_[0:P, 0:nb].unsqueeze(2).to_broadcast([P, nb, 8])
                k.I("dve", "tensor_reduce", [ltok], [m1], out=m1[0:P, 0:nb], in_=ltok[0:P, 0:nb, :], axis=AX.X, op=ALU.max)
                k.tt(t8[0:P, 0:nb, :], ltok[0:P, 0:nb, :], bc(m1), ALU.is_equal, [ltok, m1], [t8])
                k.ts(t8[0:P, 0:nb, :], t8[0:P, 0:nb, :], -1e30, None, ALU.mult, None, [t8], [t8])
                k.tt(t8[0:P, 0:nb, :], t8[0:P, 0:nb, :], ltok[0:P, 0:nb, :], ALU.add, [t8, ltok], [t8])
                k.I("dve", "tensor_reduce", [t8], [m2], out=m2[0:P, 0:nb], in_=t8[0:P, 0:nb, :], axis=AX.X, op=ALU.max)
                k.tt(t8[0:P, 0:nb, :], ltok[0:P, 0:nb, :], bc(m2), ALU.is_ge, [ltok, m2], [t8])
                k.tt(t8b[0:P, 0:nb, :], ltok[0:P, 0:nb, :], bc(m1), ALU.subtract, [ltok, m1], [t8b])
                k.act(t8b[0:P, 0:nb, :], t8b[0:P, 0:nb, :], AF.Exp, [t8b], [t8b])
                k.tt(t8b[0:P, 0:nb, :], t8b[0:P, 0:nb, :], t8[0:P, 0:nb, :], ALU.mult, [t8b, t8], [t8b])
                k.I("dve", "tensor_reduce", [t8b], [m2], out=m2[0:P, 0:nb], in_=t8b[0:P, 0:nb, :], axis=AX.X, op=ALU.add)
                k.I("dve", "reciprocal", [m2], [m2], out=m2[0:P, 0:nb], in_=m2[0:P, 0:nb])
                k.tt(t8b[0:P, 0:nb, :], t8b[0:P, 0:nb, :], bc(m2), ALU.mult, [t8b, m2], [t8b])
                for b in range(nb):
                    w_ = min(128, nt - b * 128)
                    ps2 = k.ps()
                    k.tr(ps2[0:8, 0:w_], t8b[0:w_, b, :], ident[0:w_, 0:w_], [t8b, ident], [ps2])
                    k.copy(gT[:, b * 128:b * 128 + w_], ps2[0:8, 0:w_], [ps2], [gT])
                for e in range(8):
                    ps2 = k.ps()
                    k.mm(ps2[:, 0:nt], selE[0:8, e, :], gT[0:8, 0:nt], [selE, gT], [ps2])
                    k.copy(gbc[:, e, 0:nt], ps2[:, 0:nt], [ps2], [gbc], eng="act")

            for e in range(8):
                for hc in range(24):
                    i = cnt[0]; cnt[0] += 1
                    sa = gs[i % 2]

                    def epi_a(ps, sa=sa, nt=nt, e=e):
                        k.act(sa[:, 0:nt], ps[:, 0:nt], AF.Silu, [ps], [sa])
                        k.tt(sa[:, 0:nt], sa[:, 0:nt], gbc[:, e, 0:nt], ALU.mult, [sa, gbc], [sa], eng="pool")

                    def epi_b(ps, sa=sa, hc=hc, nt=nt):
                        k.tt(HID[:, hc, 0:nt], ps[:, 0:nt], sa[:, 0:nt], ALU.mult, [ps, sa], [HID])
                    jobs.append(dict(pre=pre_moe if (e == 0 and hc == 0) else None,
                                     pieces=[(wview(mw1[e], 0, KC, hc * 128, 128), KC, 128)], x=U, nt=nt, epi=epi_a))
                    jobs.append(dict(pieces=[(wview(mw3[e], 0, KC, hc * 128, 128), KC, 128)], x=U, nt=nt, epi=epi_b))
                for oc in range(KC):
                    def epi_f(ps, oc=oc, nt=nt, e=e):
                        if e == 0:
                            k.copy(ACC[:, oc, 0:nt], ps[:, 0:nt], [ps], [ACC], eng="act")
                        else:
                            k.tt(ACC[:, oc, 0:nt], ACC[:, oc, 0:nt], ps[:, 0:nt], ALU.add, [ACC, ps], [ACC])
                    jobs.append(dict(pieces=[(wview(mw2[e], 0, KC, oc * 128, 128), KC, 128), (wview(mw2[e], KC, 8, oc * 128, 128), 8, 128)],
                                     x=HID, nt=nt, epi=epi_f))
        run_jobs(k, ws, jobs)
        if kind == "moe":
            for oc in range(KC):
                k.stt(H[:, oc, 0:nt], ACC[:, oc, 0:nt], mod[:, GF * KC + oc, j:j + 1], H[:, oc, 0:nt], ALU.mult, ALU.add,
                      [ACC, mod, H], [H])
        if final_norm:
            norm_mod(k, H, nt, ones, nfi, None, 0, None, big32, ssum, rstd, out32=OUT)
            k.dma(tview(outT, t0, nt), OUT[:, :, 0:nt], r=[OUT])
        else:
            k.dma(tview(outT, t0, nt), H[:, :, 0:nt], r=[H])
    k.finish()
    return nc


def ssd_phase(k, TC, TL, pT, yT, prm, ident, ones, one_t):
    T = TC + TL
    xbc_s = k.dram("xbc_s", [6 * 128, T], F32)
    dti_s = k.dram("dti_s", [2, 2, 8, T], F32)
    yf_s = k.dram("yf_s", [T, 512], F32)
    segs = [(0, TC), (TC, T)]
    with contextlib.ExitStack() as st:
        cw = k.sb([128, 6, 4], F32, "scw", st); cb = k.sb([128, 6], F32, "scb", st)
        k.dma(cw[:], prm["ssm_cw"], w=[cw]); k.dma(cb[:], prm["ssm_cb"], w=[cb])
        pin = [k.sb([128, T], F32, "pin", st) for _ in range(2)]
        xo = [k.sb([128, T], F32, "xo", st) for _ in range(2)]
        for blk in range(6):
            ch = CHI["sx0"] + blk
            p_ = pin[blk % 2]; x_ = xo[blk % 2]
            k.dma(p_[:], pT[ch * 128:(ch + 1) * 128, :], w=[p_], q="sp")
            conv_seg(k, x_, p_, (cw, cw[:, blk, :], 0), (cb, cb, blk), segs)
            k.act(x_[:], x_[:], AF.Silu, [x_], [x_])
            k.dma(xbc_s[blk * 128:(blk + 1) * 128, :], x_[:], r=[x_], q="pool")
        k.barrier()
    with contextlib.ExitStack() as st:
        PW = min(T, 2048)
        al = k.sb([8, 2], F32, "al", st); dtb = k.sb([8, 2], F32, "dtb", st)
        k.dma(al[:], prm["ssm_A"], w=[al]); k.dma(dtb[:], prm["ssm_dtb"], w=[dtb])
        k.act(al[:], al[:], AF.Exp, [al], [al])
        k.ts(al[:], al[:], -1.0, None, ALU.mult, None, [al], [al])
        mf = k.sb([8, PW], F32, "mf", st); mb = k.sb([8, PW], F32, "mb", st)
        k.memset(mf, 1.0); k.memset(mb, 1.0)
        k.I("pool", "memset", [], [mf], mf[:, 0::128], 0.0)
        k.I("pool", "memset", [], [mb], mb[:, 127::128], 0.0)
        xx = k.sb([8, PW], F32, "dxx", st); dt_ = k.sb([8, PW], F32, "ddt", st); cum = k.sb([8, PW], F32, "dcum", st)
        for d in range(2):
            ch = CHI["dt0"] + d
            for p0 in range(0, T, PW):
                n = min(PW, T - p0)
                k.dma(xx[:, 0:n], pT[ch * 128:ch * 128 + 8, p0:p0 + n], w=[xx])
                k.act(xx[:, 0:n], xx[:, 0:n], AF.Exp, [xx, dtb], [xx], bias=dtb[:, d:d + 1])
                k.act(dt_[:, 0:n], xx[:, 0:n], AF.Ln, [xx], [dt_], bias=one_t[0:8, 0:1])
                k.ts(xx[:, 0:n], dt_[:, 0:n], al[:, d:d + 1], None, ALU.mult, None, [dt_, al], [xx])
                if d == 0:
                    k.I("dve", "tensor_tensor_scan", [mf, xx], [cum], out=cum[:, 0:n], data0=mf[:, 0:n], data1=xx[:, 0:n],
                        initial=0.0, op0=ALU.mult, op1=ALU.add)
                else:
                    k.I("dve", "tensor_tensor_scan", [mb, xx], [cum], out=cum[:, n - 1::-1], data0=mb[:, n - 1::-1], data1=xx[:, n - 1::-1],
                        initial=0.0, op0=ALU.mult, op1=ALU.add)
                k.act(dt_[:, 0:n], dt_[:, 0:n], AF.Ln, [dt_], [dt_])
                k.tt(dt_[:, 0:n], dt_[:, 0:n], cum[:, 0:n], ALU.subtract, [dt_, cum], [dt_])
                k.dma(dti_s[d, 0, :, p0:p0 + n], dt_[:, 0:n], r=[dt_]); k.dma(dti_s[d, 1, :, p0:p0 + n], cum[:, 0:n], r=[cum])
        k.barrier()
    with contextlib.ExitStack() as st:
        maskf = k.sb([128, 128], F32, "maskf", st); maskb = k.sb([128, 128], F32, "maskb", st)
        k.memset(maskf, 1.0); k.memset(maskb, 1.0)
        k.I("pool", "affine_select", [maskf], [maskf], out=maskf[:], in_=maskf[:], pattern=[[1, 128]], compare_op=ALU.is_ge, fill=0.0, base=0, channel_multiplier=-1)
        k.I("pool", "affine_select", [maskb], [maskb], out=maskb[:], in_=maskb[:], pattern=[[-1, 128]], compare_op=ALU.is_ge, fill=0.0, base=0, channel_multiplier=1)
        BM = k.sb([40, 8, 128], F32, "BM", st)
        k.memset(BM, 1.0)
        k.I("pool", "affine_select", [BM], [BM], out=BM[0:8], in_=BM[0:8], pattern=[[-1, 8], [0, 128]], compare_op=ALU.is_equal, fill=0.0, base=0, channel_multiplier=1)
        k.dma(BM[32:40], BM[0:8], r=[BM], w=[BM])
        Lt = [k.sb([40, 128], F32, "Lt", st) for _ in range(2)]; Ct = [k.sb([40, 128], F32, "Ct", st) for _ in range(2)]
        Rt = [k.sb([40, 8, 128], F32, "Rt", st) for _ in range(2)]
        for i in range(2):
            k.memset(Lt[i], 0.0); k.memset(Ct[i], 0.0); k.memset(Rt[i], 0.0)
            k.I("pool", "memset", [], [Lt[i]], Lt[i][32:40, :], 1.0)
            k.copy(Rt[i][0:8], BM[0:8], [BM], [Rt[i]], eng="pool")
        XBC = [k.sb([128, 6, 128], F32, "XBC", st) for _ in range(2)]
        xtok = k.sb([128, 512], F32, "xtok", st); btok = k.sb([128, 128], F32, "btok", st)
        CBm = k.sb([128, 128], F32, "CBm", st); E = k.sb([128, 8, 128], F32, "E", st); S = k.sb([128, 8, 128], F32, "S", st)
        xw = k.sb([128, 512], F32, "xw", st); ecum = k.sb([128, 8], F32, "ecum", st); dc = k.sb([128, 8], F32, "dc", st)
        dg = k.sb([40, 8], F32, "dg", st)
        ych = [k.sb([128, 512], F32, "ych", st) for _ in range(2)]; yo = k.sb([128, 512], F32, "yo", st)
        Hst = k.sb([128, 512], F32, "Hst", st)
        yfl = k.sb([128, 512], F32, "yfl", st); zT = k.sb([128, 4, 128], F32, "zT", st); yg = k.sb([128, 512], F32, "yg", st)
        junk = k.sb([128, 512], F32, "junk", st); ss = k.sb([128, 1], F32, "ss", st)
        nw = k.sb([128, 512], F32, "nw", st); dsk = k.sb([128, 8], F32, "dsk", st); yot = k.sb([128, 4, 128], F32, "yot", st)
        k.dma(nw[:], prm["ssm_nw"][0:1, :].partition_broadcast(128), w=[nw])
        k.dma(dsk[:], prm["ssm_dsk"][0:1, :].partition_broadcast(128), w=[dsk])
        cchunks = list(range(0, TC, 128)); lchunks = list(range(TC, T, 128))
        it = 0
        for d in range(2):
            order = (cchunks + lchunks) if d == 0 else (cchunks[::-1] + lchunks[::-1])
            mask = maskf if d == 0 else maskb
            last = 127 if d == 0 else 0
            k.memset(Hst, 0.0, eng="dve")
            for t0 in order:
                X = XBC[it % 2]; L = Lt[it % 2]; C = Ct[it % 2]; R = Rt[it % 2]; Y = ych[it % 2]; it += 1
                k.dma(X[:], xbc_s[:, t0:t0 + 128].rearrange("(b p) t -> p b t", p=128), w=[X], q="sp")
                k.dma(L[0:8, :], dti_s[d, 0, :, t0:t0 + 128], w=[L], q="pool")
                k.dma(C[32:40, :], dti_s[d, 1, :, t0:t0 + 128], w=[C], q="pool")
                pst = k.ps()
                for b in range(4):
                    k.tr(pst[:, b * 128:(b + 1) * 128], X[:, b, :], ident[:, :], [X, ident], [pst])
                k.copy(xtok[:], pst[:, :], [pst], [xtok], eng="act")
                psb = k.ps()
                k.tr(psb[:, 0:128], X[:, 4, :], ident[:, :], [X, ident], [psb])
                k.copy(btok[:], psb[:, 0:128], [psb], [btok], eng="act")
                pcb = k.ps()
                k.mm(pcb[:, 0:128], X[:, 4, :], X[:, 5, :], [X], [pcb])
                k.tt(CBm[:], pcb[:, 0:128], mask[:], ALU.mult, [pcb, mask], [CBm])
                k.tt(R[32:40], C[32:40, :].unsqueeze(1).to_broadcast([8, 8, 128]), BM[32:40], ALU.mult, [C, BM], [R])
                for hh in range(2):
                    psg = k.ps()
                    k.mm(psg[:, :], L[0:40, :], R[0:40, hh * 4:(hh + 1) * 4, :], [L, R], [psg])
                    k.ts(E[:, hh * 4:(hh + 1) * 4, :], psg[:, :].rearrange("p (a b) -> p a b", a=4), 30.0, None, ALU.min, None, [psg], [E])
                k.act(E[:], E[:], AF.Exp, [E], [E])
                k.tt(S[:], E[:], CBm[:].unsqueeze(1).to_broadcast([128, 8, 128]), ALU.mult, [E, CBm], [S])
                pc1 = k.ps()
                k.mm(pc1[:, 0:8], C[32:40, :], ident[32:40, 32:40], [C, ident], [pc1])
                k.act(ecum[:], pc1[:, 0:8], AF.Exp, [pc1], [ecum])
                k.ts(dg[32:40, :], ident[32:40, 32:40], C[32:40, last:last + 1], None, ALU.mult, None, [ident, C], [dg])
                pc2 = k.ps()
                k.mm(pc2[:, 0:8], L[32:40, :], dg[32:40, :], [L, dg], [pc2])
                k.act(dc[:], pc2[:, 0:8], AF.Exp, [pc2], [dc])
                k.tt(xw[:].rearrange("p (h q) -> p h q", h=8), xtok[:].rearrange("p (h q) -> p h q", h=8),
                     E[:, :, last:last + 1].to_broadcast([128, 8, 64]), ALU.mult, [xtok, E], [xw])
                pss = k.ps()
                k.mm(pss[:, :], btok[:], xw[:], [btok, xw], [pss])
                pyd = k.ps()
                for h in range(8):
                    k.mm(pyd[:, h * 64:(h + 1) * 64], S[:, h, :], xtok[:, h * 64:(h + 1) * 64], [S, xtok], [pyd])
                pyo = k.ps()
                k.mm(pyo[:, :], X[:, 5, :], Hst[:], [X, Hst], [pyo])
                k.tt(yo[:].rearrange("p (h q) -> p h q", h=8), pyo[:, :].rearrange("p (h q) -> p h q", h=8),
                     ecum[:].unsqueeze(2).to_broadcast([128, 8, 64]), ALU.mult, [pyo, ecum], [yo])
                k.tt(Y[:], yo[:], pyd[:, :], ALU.add, [yo, pyd], [Y])
                k.tt(Hst[:].rearrange("p (h q) -> p h q", h=8), Hst[:].rearrange("p (h q) -> p h q", h=8),
                     dc[:].unsqueeze(2).to_broadcast([128, 8, 64]), ALU.mult, [Hst, dc], [Hst])
                k.tt(Hst[:], Hst[:], pss[:, :], ALU.add, [Hst, pss], [Hst])
                if d == 0:
                    k.dma(yf_s[t0:t0 + 128, :], Y[:], r=[Y], q="pool")
                else:
                    k.dma(yfl[:], yf_s[t0:t0 + 128, :], w=[yfl], q="sp")
                    k.dma(zT[:], pT[CHI["z0"] * 128:(CHI["z0"] + 4) * 128, t0:t0 + 128].rearrange("(b p) t -> p b t", p=128), w=[zT], q="sp")
                    k.tt(Y[:], Y[:], yfl[:], ALU.add, [Y, yfl], [Y])
                    k.tt(yo[:].rearrange("p (h q) -> p h q", h=8), xtok[:].rearrange("p (h q) -> p h q", h=8),
                         dsk[:].unsqueeze(2).to_broadcast([128, 8, 64]), ALU.mult, [xtok, dsk], [yo])
                    k.tt(Y[:], Y[:], yo[:], ALU.add, [Y, yo], [Y])
                    k.act(zT[:], zT[:], AF.Silu, [zT], [zT])
                    pz = k.ps()
                    for b in range(4):
                        k.tr(pz[:, b * 128:(b + 1) * 128], zT[:, b, :], ident[:, :], [zT, ident], [pz])
                    k.tt(yg[:], Y[:], pz[:, :], ALU.mult, [Y, pz], [yg])
                    k.tt(junk[:], yg[:], yg[:], ALU.mult, [yg], [junk])
                    k.I("dve", "tensor_reduce", [junk], [ss], out=ss[:], in_=junk[:], axis=AX.X, op=ALU.add)
                    k.act(ss[:], ss[:], AF.Sqrt, [ss], [ss], scale=1.0 / 512, bias=k.eps_t[:, 0:1])
                    k.I("dve", "reciprocal", [ss], [ss], out=ss[:], in_=ss[:])
                    k.stt(yg[:], yg[:], ss[:, 0:1], nw[:], ALU.mult, ALU.mult, [yg, ss, nw], [yg])
                    po = k.ps()
                    for b in range(4):
                        k.tr(po[:, b * 128:(b + 1) * 128], yg[:, b * 128:(b + 1) * 128], ident[:, :], [yg, ident], [po])
                    k.copy(yot[:].rearrange("p b t -> p (b t)"), po[:, :], [po], [yot], eng="act")
                    k.dma(yT[2, :, t0:t0 + 128].rearrange("(b p) t -> p b t", p=128), yot[:], r=[yot], q="pool")
        k.barrier()


RCH = ["r0", "r1", "r2", "r3", "k0", "k1", "k2", "k3", "v0", "v1", "v2", "v3", "wlo0", "wlo1", "alo0", "alo1", "glo0", "glo1"]


def rwkv_phase(k, TC, TL, pT, yT, prm, ident, ones, one_t):
    T = TC + TL
    NCHK = T // 128
    rw_s = k.dram("rw_s", [2, 4, 4, 128, T], F32)
    vs_s = k.dram("vs_s", [4, 128, T], F32)
    pp_s = k.dram("pp_s", [2, 4, 128, T], F32)
    yr_s = k.dram("yr_s", [2, 4, T, 128], F32)
    gend = k.sb([128, 2, 4, NCHK], F32, "gend")
    gend_s = k.dram("gend_s", [128, 2 * 4 * NCHK], F32)
    gend2 = k.sb([64, 2, 2 * 4 * NCHK], F32, "gend2")
    blk1 = k.sb([128, 128], F32, "blk1")
    k.memset(blk1, 0.0)
    k.I("pool", "memset", [], [blk1], blk1[0:64, 0:64], 1.0)
    k.I("pool", "memset", [], [blk1], blk1[64:128, 64:128], 1.0)
    with contextlib.ExitStack() as st:
        PW = 512
        mu = k.sb([128, 18], F32, "mu", st); om = k.sb([128, 18], F32, "om", st); hm = k.sb([128, 18], F32, "hm", st)
        k.dma(mu[:], prm["rw_mu"], w=[mu])
        k.ts(om[:], mu[:], -1.0, 1.0, ALU.mult, ALU.add, [mu], [om])
        k.ts(hm[:], mu[:], 0.5, None, ALU.mult, None, [mu], [hm])
        w0 = k.sb([128, 2, 4], F32, "w0", st); a0 = k.sb([128, 2, 4], F32, "a0", st)
        kk_ = k.sb([128, 4], F32, "kk_", st); ka = k.sb([128, 4], F32, "ka", st); omka = k.sb([128, 4], F32, "omka", st); rk = k.sb([128, 4], F32, "rk", st)
        for t_, n_ in ((w0, "rw_w0"), (a0, "rw_a0"), (kk_, "rw_kk"), (ka, "rw_ka"), (rk, "rw_rk")):
            k.dma(t_[:], prm[n_], w=[t_])
        k.ts(omka[:], ka[:], -1.0, 1.0, ALU.mult, ALU.add, [ka], [omka])
        wup = k.sb([96, 2, 512], F32, "wup", st); aup = k.sb([96, 2, 512], F32, "aup", st); gup = k.sb([128, 2, 512], F32, "gup", st)
        k.dma(wup[:], prm["rw_wup"].rearrange("d k n -> k d n"), w=[wup]); k.dma(aup[:], prm["rw_aup"].rearrange("d k n -> k d n"), w=[aup])
        k.dma(gup[:], prm["rw_gup"].rearrange("(c p) n -> p c n", p=128), w=[gup])
        e12 = k.sb([128, 1], F32, "e12", st); k.memset(e12, 1e-12)
        mf = k.sb([128, PW], F32, "mf", st); mb = k.sb([128, PW], F32, "mb", st)
        k.memset(mf, 1.0); k.memset(mb, 1.0)
        k.I("pool", "memset", [], [mf], mf[:, 0::128], 0.0)
        k.I("pool", "memset", [], [mb], mb[:, 127::128], 0.0)
        Pin = [k.sb([128, PW + 2], F32, "Pin", st) for _ in range(3)]
        SH = [k.sb([128, PW], F32, "SH", st) for _ in range(18)]
        tmp = k.sb([128, PW], F32, "tmp", st)
        W = lambda nm: k.sb([128, PW], F32, nm, st)
        gT = W("gT"); kkn = W("kkn"); t1 = W("t1"); t2 = W("t2"); lw = W("lw"); lg = W("lg"); alr = W("alr"); key = W("key"); ks = W("ks")
        eneg = W("eneg"); epos = W("epos"); eex = W("eex")
        outs = [[W("o%d%d" % (i, j)) for j in range(4)] for i in range(2)]
        bon = W("bon")
        oi = 0
        for (s, n, isc) in segs_tiles(TC, TL, PW):
            s0, s1 = (0, TC) if isc else (TC, T)
            for ci, cn in enumerate(RCH):
                ch = CHI[cn]; m = CH[ch][1]
                P = Pin[ci % 3]
                lo = max(s0, s - 1); hi = min(s1, s + n + 1)
                k.dma(P[0:m, lo - (s - 1):hi - (s - 1)], pT[ch * 128:ch * 128 + m, lo:hi], w=[P], q="sp" if ci % 2 else "pool")
                if s - 1 < s0:
                    k.I("dve", "memset", [], [P], P[0:m, 0:1], 0.0)
                if s + n + 1 > s1:
                    k.I("dve", "memset", [], [P], P[0:m, n + 1:n + 2], 0.0)
                k.tt(tmp[0:m, 0:n], P[0:m, 0:n], P[0:m, 2:n + 2], ALU.add, [P], [tmp])
                k.ts(SH[ci][0:m, 0:n], P[0:m, 1:n + 1], om[0:m, ci:ci + 1], None, ALU.mult, None, [P, om], [SH[ci]])
                k.stt(SH[ci][0:m, 0:n], tmp[0:m, 0:n], hm[0:m, ci:ci + 1], SH[ci][0:m, 0:n], ALU.mult, ALU.add, [tmp, hm, SH[ci]], [SH[ci]])
            for ci in (12, 13):
                k.act(SH[ci][0:96, 0:n], SH[ci][0:96, 0:n], AF.Tanh, [SH[ci]], [SH[ci]])
            for ci in (16, 17):
                k.act(SH[ci][:, 0:n], SH[ci][:, 0:n], AF.Sigmoid, [SH[ci]], [SH[ci]])
            for hb in range(4):
                R_, K_, V_ = SH[hb], SH[4 + hb], SH[8 + hb]
                cs_ = slice(hb * 128, (hb + 1) * 128)
                ps = k.ps()
                for kc in range(2):
                    k.mm(ps[:, 0:n], gup[:, kc, cs_], SH[16 + kc][:, 0:n], [gup, SH[16 + kc]], [ps], start=(kc == 0), stop=(kc == 1))
                k.copy(gT[:, 0:n], ps[:, 0:n], [ps], [gT], eng="act")
                k.dma(pp_s[0, hb, :, s:s + n], gT[:, 0:n], r=[gT], q="pool")
                k.dma(vs_s[hb, :, s:s + n], V_[:, 0:n], r=[V_], q="pool")
                k.ts(t1[:, 0:n], K_[:, 0:n], kk_[:, hb:hb + 1], None, ALU.mult, None, [K_, kk_], [t1])
                k.tt(t2[:, 0:n], t1[:, 0:n], t1[:, 0:n], ALU.mult, [t1], [t2])
                ps = k.ps()
                k.mm(ps[:, 0:n], blk1[:], t2[:, 0:n], [blk1, t2], [ps])
                k.act(t2[:, 0:n], ps[:, 0:n], AF.Sqrt, [ps, e12], [t2], bias=e12[:, 0:1])
                k.I("dve", "reciprocal", [t2], [t2], out=t2[:, 0:n], in_=t2[:, 0:n])
                k.tt(kkn[:, 0:n], t1[:, 0:n], t2[:, 0:n], ALU.mult, [t1, t2], [kkn])
                for d in range(2):
                    ps = k.ps()
                    k.mm(ps[:, 0:n], wup[0:96, d, cs_], SH[12 + d][0:96, 0:n], [wup, SH[12 + d]], [ps])
                    k.act(lw[:, 0:n], ps[:, 0:n], AF.Sigmoid, [ps, w0], [lw], bias=w0[:, d, hb:hb + 1])
                    k.ts(lw[:, 0:n], lw[:, 0:n], -0.6065306597126334, None, ALU.mult, None, [lw], [lw])
                    if d == 0:
                        k.I("dve", "tensor_tensor_scan", [mf, lw], [lg], out=lg[:, 0:n], data0=mf[:, 0:n], data1=lw[:, 0:n], initial=0.0, op0=ALU.mult, op1=ALU.add)
                    else:
                        k.I("dve", "tensor_tensor_scan", [mb, lw], [lg], out=lg[:, n - 1::-1], data0=mb[:, n - 1::-1], data1=lw[:, n - 1::-1], initial=0.0, op0=ALU.mult, op1=ALU.add)
                    ps = k.ps()
                    k.mm(ps[:, 0:n], aup[0:96, d, cs_], SH[14 + d][0:96, 0:n], [aup, SH[14 + d]], [ps])
                    k.act(alr[:, 0:n], ps[:, 0:n], AF.Sigmoid, [ps, a0], [alr], bias=a0[:, d, hb:hb + 1])
                    k.ts(t1[:, 0:n], alr[:, 0:n], ka[:, hb:hb + 1], omka[:, hb:hb + 1], ALU.mult, ALU.add, [alr, ka, omka], [t1])
                    k.tt(key[:, 0:n], t1[:, 0:n], K_[:, 0:n], ALU.mult, [t1, K_], [key])
                    if d == 0:
                        k.copy(ks[:, 0:n], key[:, 0:n], [key], [ks], eng="pool")
                    else:
                        k.tt(ks[:, 0:n], ks[:, 0:n], key[:, 0:n], ALU.add, [ks, key], [ks], eng="pool")
                    k.act(eneg[:, 0:n], lg[:, 0:n], AF.Exp, [lg], [eneg], scale=-1.0)
                    k.act(epos[:, 0:n], lg[:, 0:n], AF.Exp, [lg], [epos])
                    k.tt(t2[:, 0:n], lg[:, 0:n], lw[:, 0:n], ALU.subtract, [lg, lw], [t2])
                    k.act(eex[:, 0:n], t2[:, 0:n], AF.Exp, [t2], [eex])
                    O = outs[oi % 2]; oi += 1
                    k.stt(O[0][:, 0:n], kkn[:, 0:n], -1.0, eex[:, 0:n], ALU.mult, ALU.mult, [kkn, eex], [O[0]])
                    k.tt(O[1][:, 0:n], R_[:, 0:n], epos[:, 0:n], ALU.mult, [R_, epos], [O[1]])
                    k.tt(t1[:, 0:n], kkn[:, 0:n], alr[:, 0:n], ALU.mult, [kkn, alr], [t1])
                    k.tt(O[2][:, 0:n], t1[:, 0:n], eneg[:, 0:n], ALU.mult, [t1, eneg], [O[2]])
                    k.tt(O[3][:, 0:n], key[:, 0:n], eneg[:, 0:n], ALU.mult, [key, eneg], [O[3]])
                    for kind in range(4):
                        k.dma(rw_s[d, hb, kind, :, s:s + n], O[kind][:, 0:n], r=[O[kind]], q=("sp", "pool")[kind % 2])
                    c0 = s // 128; ncs = n // 128
                    src = epos[:, 127:n:128] if d == 0 else epos[:, 0:n:128]
                    k.copy(gend[:, d, hb, c0:c0 + ncs], src, [epos], [gend], eng="pool")
                k.stt(t1[:, 0:n], R_[:, 0:n], rk[:, hb:hb + 1], ks[:, 0:n], ALU.mult, ALU.mult, [R_, rk, ks], [t1])
                ps = k.ps()
                k.mm(ps[:, 0:n], blk1[:], t1[:, 0:n], [blk1, t1], [ps])
                k.tt(bon[:, 0:n], ps[:, 0:n], V_[:, 0:n], ALU.mult, [ps, V_], [bon])
                k.dma(pp_s[1, hb, :, s:s + n], bon[:, 0:n], r=[bon], q="sp")
        k.dma(gend_s[:, :], gend[:].rearrange("p d b c -> p (d b c)"), r=[gend])
        k.barrier()
    k.dma(gend2[:], gend_s.rearrange("(h p) x -> p h x", h=2), w=[gend2])
    RW = "123"
    if "2" in RW:
      with contextlib.ExitStack() as st:
          def mk(name, pattern, cm, op):
              m_ = k.sb([128, 128], F32, name, st)
              k.memset(m_, 1.0)
              k.I("pool", "affine_select", [m_], [m_], out=m_[:], in_=m_[:], pattern=pattern, compare_op=op, fill=0.0, base=0, channel_multiplier=cm)
              return m_
          up_s = mk("up_s", [[1, 128]], -1, ALU.is_gt)
          up_i = mk("up_i", [[1, 128]], -1, ALU.is_ge)
          lo_s = mk("lo_s", [[-1, 128]], 1, ALU.is_gt)
          lo_i = mk("lo_i", [[-1, 128]], 1, ALU.is_ge)
          M4 = []
          for d in range(2):
              m4 = k.sb([128, 4, 128], F32, "m4_%d" % d, st)
              ms, mi = (up_s, up_i) if d == 0 else (lo_s, lo_i)
              for j, mm_ in enumerate((ms, mi, ms, mi)):
                  k.copy(m4[:, j, :], mm_[:], [mm_], [m4], eng="pool")
              M4.append(m4)
          NM = [lo_s, up_s]
          qps = []
          for j in range(4):
              for t_ in k.ps_tiles[2:]:
                  qps.append(Sub(t_, t_.t[:, j * 128:(j + 1) * 128], t_.name + "_q%d" % j))
          fps = k.ps_tiles[0:2]
          qi = [0]; fi = [0]

          def q():
              qi[0] += 1
              return qps[qi[0] % len(qps)]

          def fp():
              fi[0] += 1
              return fps[fi[0] % 2]
          chains = [(hb, d) for hb in range(4) for d in range(2)]
          cchunks = list(range(0, TC, 128)); lchunks = list(range(TC, T, 128))
          order = {0: cchunks + lchunks, 1: cchunks[::-1] + lchunks[::-1]}
          nsteps = len(order[0])
          GRP = 4
          STOP = 9
          def alloc_chain(st2):
              b = {}
              b["AR"] = [k.sb([128, 2, 2, 128], F32, "AR", st2) for _ in range(2)]
              for t_ in b["AR"]:
                  k.memset(t_, 0.0)
              b["BK2"] = [k.sb([64, 2, 2, 128], F32, "BK2", st2) for _ in range(2)]
              b["BKV"] = [k.sb([128, 3, 128], F32, "BKV", st2) for _ in range(2)]
              b["tok"] = k.sb([128, 3, 128], F32, "tok", st2)
              b["M4"] = [k.sb([128, 4, 128], F32, "M4h", st2) for _ in range(2)]
              b["N"] = [[k.sb([128, 128], F32, "N", st2) for _ in range(2)] for _ in range(2)]
              b["NT"] = [[k.sb([128, 128], F32, "NT", st2) for _ in range(2)] for _ in range(2)]
              b["X"] = [[k.sb([128, 64], F32, "X", st2) for _ in range(2)] for _ in range(2)]
              b["Y"] = k.sb([128, 128], F32, "Ych", st2)
              b["H"] = k.sb([128, 2, 64], F32, "Hst", st2)
              k.memset(b["H"], 0.0, eng="dve")
              return b
          for g0 in range(0, len(chains), GRP):
              grp = chains[g0:g0 + GRP]
              st2 = contextlib.ExitStack()
              B = {c: alloc_chain(st2) for c in grp}
              for step in range(nsteps):
                  U = {}
                  for c in grp:
                      hb, d = c; b = B[c]; t0 = order[d][step]
                      AR = b["AR"][step % 2]; BKV = b["BKV"][step % 2]
                      BK2 = b["BK2"][step % 2]
                      for h in range(2):
                          k.dma(AR[0:64, h, :, :], rw_s[d, hb, 0:2, h * 64:(h + 1) * 64, t0:t0 + 128].rearrange("k p t -> p k t"), w=[AR], q="sp")
                          k.dma(BK2[:, h, :, :], rw_s[d, hb, 2:4, h * 64:(h + 1) * 64, t0:t0 + 128].rearrange("k p t -> p k t"), w=[BK2], q="pool")
                      k.dma(BKV[:, 0:2, :], rw_s[d, hb, 2:4, :, t0:t0 + 128].rearrange("k p t -> p k t"), w=[BKV], q="pool")
                      k.dma(BKV[:, 2, :], vs_s[hb, :, t0:t0 + 128], w=[BKV], q="sp")
                  if STOP < 1: continue
                  for c in grp:
                      hb, d = c; b = B[c]
                      BKV = b["BKV"][step % 2]
                      pt = fp()
                      for j in range(3):
                          k.tr(pt[:, j * 128:(j + 1) * 128], BKV[:, j, :], ident[:, :], [BKV, ident], [pt])
                      k.copy(b["tok"][:].rearrange("p a b -> p (a b)"), pt[:, 0:384], [pt], [b["tok"]], eng="act")
                  if STOP < 2: continue
                  for c in grp:
                      hb, d = c; b = B[c]
                      AR = b["AR"][step % 2]; BK2 = b["BK2"][step % 2]
                      for h in range(2):
                          hr = slice(h * 64, (h + 1) * 64)
                          pf = fp()
                          k.mm(pf[:, 0:256], BK2[:, h, 0, :], AR[0:64, h, :, :], [BK2, AR], [pf])
                          k.mm(pf[:, 256:512], BK2[:, h, 1, :], AR[0:64, h, :, :], [BK2, AR], [pf])
                          k.tt(b["M4"][h][:].rearrange("p a b -> p (a b)"), pf[:, :], M4[d][:].rearrange("p a b -> p (a b)"), ALU.mult, [pf, M4[d]], [b["M4"][h]])
                          pq = q()
                          k.mm(pq[:, :], AR[0:64, h, 0, :], BK2[:, h, 0, :], [AR, BK2], [pq])
                          k.tt(b["N"][h][0][:], pq[:, :], NM[d][:], ALU.mult, [pq, NM[d]], [b["N"][h][0]])
                  if STOP < 3: continue
                  for c in grp:
                      hb, d = c; b = B[c]
                      AR = b["AR"][step % 2]
                      for h in range(2):
                          hr = slice(h * 64, (h + 1) * 64)
                          pq = q()
                          k.mm(pq[:, 0:64], AR[:, h, 0, :], b["H"][:, h, :], [AR, b["H"]], [pq], start=True, stop=False)
                          k.mm(pq[:, 0:64], b["M4"][h][:, 2, :], b["tok"][:, 2, hr], [b["M4"][h], b["tok"]], [pq], start=False, stop=True)
                          k.copy(b["X"][h][0][:], pq[:, 0:64], [pq], [b["X"][h][0]], eng="act")
                  if STOP < 4: continue
                  for lv in range(7):
                      for c in grp:
                          hb, d = c; b = B[c]
                          for h in range(2):
                              NTc = b["M4"][h][:, 0, :] if lv == 0 else b["NT"][h][lv % 2][:]
                              NTt = b["M4"][h] if lv == 0 else b["NT"][h][lv % 2]
                              Nc = b["N"][h][lv % 2]
                              Xc = b["X"][h][lv % 2]; Xn = b["X"][h][(lv + 1) % 2]
                              pq = q()
                              k.mm(pq[:, 0:64], NTc, Xc[:], [NTt, Xc], [pq])
                              k.tt(Xn[:], Xc[:], pq[:, 0:64], ALU.add, [Xc, pq], [Xn])
                              if lv < 6:
                                  Nn = b["N"][h][(lv + 1) % 2]; NTn = b["NT"][h][(lv + 1) % 2]
                                  p1 = q()
                                  k.mm(p1[:, :], NTc, Nc[:], [NTt, Nc], [p1])
                                  k.copy(Nn[:], p1[:, :], [p1], [Nn], eng="act")
                                  p2 = q()
                                  k.mm(p2[:, :], Nc[:], NTc, [Nc, NTt], [p2])
                                  k.copy(NTn[:], p2[:, :], [p2], [NTn], eng=("dve", "act")[h])
                  if STOP < 5: continue
                  for c in grp:
                      hb, d = c; b = B[c]; t0 = order[d][step]
                      AR = b["AR"][step % 2]
                      py = q(); phs = q()
                      for h in range(2):
                          hr = slice(h * 64, (h + 1) * 64)
                          Uh = b["X"][h][1]
                          k.mm(py[:, hr], AR[:, h, 1, :], b["H"][:, h, :], [AR, b["H"]], [py], start=True, stop=False)
                          k.mm(py[:, hr], b["M4"][h][:, 1, :], Uh[:], [b["M4"][h], Uh], [py], start=False, stop=False)
                          k.mm(py[:, hr], b["M4"][h][:, 3, :], b["tok"][:, 2, hr], [b["M4"][h], b["tok"]], [py], start=False, stop=True)
                          k.mm(phs[0:64, hr], b["tok"][:, 0, hr], Uh[:], [b["tok"], Uh], [phs], start=True, stop=False)
                          k.mm(phs[0:64, hr], b["tok"][:, 1, hr], b["tok"][:, 2, hr], [b["tok"]], [phs], start=False, stop=True)
                      k.copy(b["Y"][:], py[:, :], [py], [b["Y"]], eng="act")
                      k.dma(yr_s[d, hb, t0:t0 + 128, :], b["Y"][:], r=[b["Y"]], q="pool")
                      k.tt(b["H"][0:64], b["H"][0:64], phs[0:64, :].rearrange("p (h v) -> p h v", h=2), ALU.add, [b["H"], phs], [b["H"]])
                      gi = (d * 4 + hb) * NCHK + t0 // 128
                      k.tt(b["H"][0:64], b["H"][0:64], gend2[:, :, gi:gi + 1].to_broadcast([64, 2, 64]), ALU.mult, [b["H"], gend2], [b["H"]])
              k.barrier()
              st2.close()
          k.barrier()
    if "3" in RW:
      with contextlib.ExitStack() as st:
          lnw = k.sb([128, 4], F32, "lnw", st); lnb = k.sb([128, 4], F32, "lnb", st)
          k.dma(lnw[:], prm["rw_lnw"], w=[lnw]); k.dma(lnb[:], prm["rw_lnb"], w=[lnb])
          egn = k.sb([128, 1], F32, "egn", st); k.memset(egn, 64e-5)
          NB_ = 4
          Y0 = [k.sb([128, NB_, 128], F32, "Y0", st) for _ in range(2)]; Y1 = [k.sb([128, NB_, 128], F32, "Y1", st) for _ in range(2)]
          GB = [k.sb([128, 2, NB_ * 128], F32, "GB", st) for _ in range(2)]
          s1 = k.sb([128, NB_ * 2], F32, "s1", st); s2 = k.sb([128, NB_ * 2], F32, "s2", st)
          yc = k.sb([128, NB_, 128], F32, "yc", st); sq = k.sb([128, NB_, 128], F32, "sq", st)
          OT = [k.sb([128, NB_ * 128], F32, "OT", st) for _ in range(2)]
          it = 0
          for hb in range(4):
              for t0 in range(0, T, NB_ * 128):
                  nb = min(NB_, (T - t0) // 128); n = nb * 128
                  a, b_, gb, ot = Y0[it % 2], Y1[it % 2], GB[it % 2], OT[it % 2]; it += 1
                  k.dma(a[:, 0:nb, :], yr_s[0, hb, t0:t0 + n, :].rearrange("(c p) v -> p c v", p=128), w=[a], q="sp")
                  k.dma(b_[:, 0:nb, :], yr_s[1, hb, t0:t0 + n, :].rearrange("(c p) v -> p c v", p=128), w=[b_], q="pool")
                  k.dma(gb[:, :, 0:n], pp_s[:, hb, :, t0:t0 + n].rearrange("k p t -> p k t"), w=[gb], q="sp")
                  k.tt(a[:, 0:nb, :], a[:, 0:nb, :], b_[:, 0:nb, :], ALU.add, [a, b_], [a])
                  v4 = lambda t_: t_[:, 0:nb, :].rearrange("p c (h v) -> p (c h) v", h=2)
                  k.I("dve", "tensor_reduce", [a], [s1], out=s1[:, 0:nb * 2], in_=v4(a), axis=AX.X, op=ALU.add)
                  k.ts(s1[:, 0:nb * 2], s1[:, 0:nb * 2], 1.0 / 64, None, ALU.mult, None, [s1], [s1])
                  k.tt(v4(yc), v4(a), s1[:, 0:nb * 2].unsqueeze(2).to_broadcast([128, nb * 2, 64]), ALU.subtract, [a, s1], [yc])
                  k.tt(sq[:, 0:nb, :], yc[:, 0:nb, :], yc[:, 0:nb, :], ALU.mult, [yc], [sq])
                  k.I("dve", "tensor_reduce", [sq], [s2], out=s2[:, 0:nb * 2], in_=v4(sq), axis=AX.X, op=ALU.add)
                  k.act(s2[:, 0:nb * 2], s2[:, 0:nb * 2], AF.Sqrt, [s2, egn], [s2], scale=1.0 / 64, bias=egn[:, 0:1])
                  k.I("dve", "reciprocal", [s2], [s2], out=s2[:, 0:nb * 2], in_=s2[:, 0:nb * 2])
                  k.tt(v4(yc), v4(yc), s2[:, 0:nb * 2].unsqueeze(2).to_broadcast([128, nb * 2, 64]), ALU.mult, [yc, s2], [yc])
                  pt = k.ps_tiles[it % 2]
                  for c in range(nb):
                      k.tr(pt[:, c * 128:(c + 1) * 128], yc[:, c, :], ident[:, :], [yc, ident], [pt])
                  k.ts(ot[:, 0:n], pt[:, 0:n], lnw[:, hb:hb + 1], lnb[:, hb:hb + 1], ALU.mult, ALU.add, [pt, lnw, lnb], [ot])
                  k.tt(ot[:, 0:n], ot[:, 0:n], gb[:, 1, 0:n], ALU.add, [ot, gb], [ot])
                  k.tt(ot[:, 0:n], ot[:, 0:n], gb[:, 0, 0:n], ALU.mult, [ot, gb], [ot])
                  k.dma(yT[1, hb * 128:(hb + 1) * 128, t0:t0 + n], ot[:, 0:n], r=[ot], q="pool")
          k.barrier()


CH = ([("lx%d" % i, 128) for i in range(4)] + [("lg%d" % i, 128) for i in range(4)] +
      [("r%d" % i, 128) for i in range(4)] + [("k%d" % i, 128) for i in range(4)] + [("v%d" % i, 128) for i in range(4)] +
      [("wlo0", 96), ("wlo1", 96), ("alo0", 96), ("alo1", 96), ("glo0", 128), ("glo1", 128)] +
      [("z%d" % i, 128) for i in range(4)] + [("sx%d" % i, 128) for i in range(4)] + [("sB", 128), ("sC", 128), ("dt0", 8), ("dt1", 8)])
CHI = {n: i for i, (n, w) in enumerate(CH)}
CHOFF = np.cumsum([0] + [w for n, w in CH]).tolist()
NCOL = CHOFF[-1]
NCH = len(CH)


def segs_tiles(TC, TL, n):
    out = [(s, min(n, TC - s), 1) for s in range(0, TC, n)]
    out += [(TC + s, min(n, TL - s), 0) for s in range(0, TL, n)]
    return out


def conv_seg(k, out, p, cw, cb, segs, eng="dve"):
    w = lambda i: cw[1][:, cw[2] + i:cw[2] + i + 1]
    for (s, e) in segs:
        k.ts(out[:, s:e], p[:, s:e], w(2), cb[1][:, cb[2]:cb[2] + 1], ALU.mult, ALU.add, [p, cw[0], cb[0]], [out])
        k.stt(out[:, s + 2:e], p[:, s:e - 2], w(0), out[:, s + 2:e], ALU.mult, ALU.add, [p, cw[0], out], [out])
        k.stt(out[:, s + 1:e], p[:, s:e - 1], w(1), out[:, s + 1:e], ALU.mult, ALU.add, [p, cw[0], out], [out])
        k.stt(out[:, s:e - 1], p[:, s + 1:e], w(3), out[:, s:e - 1], ALU.mult, ALU.add, [p, cw[0], out], [out])


def build_A(TC, TL, do=("lru", "ssd", "rwkv"), dbg_p=False):
    T = TC + TL
    nc = bass.Bass("TRN2", target_bir_lowering=False)
    k = KB(nc)
    inp = lambda n, s: k.dram(n, s, F32, "ExternalInput")
    hT = inp("hT", [D, T]); cT = inp("cT", [D, 2]); ada_w = inp("ada_w", [D, 2 * D]); ada_b_l = inp("ada_b_l", [128, 32])
    nmix = inp("nmix", [128, KC]); w_sel = inp("w_sel", [D, NCOL])
    lru_cw = inp("lru_cw", [128, 4, 4]); lru_cb = inp("lru_cb", [128, 4]); lru_gw = inp("lru_gw", [2, 2, 4, 128, 128])
    lru_gb = inp("lru_gb", [128, 2, 2, 4]); lru_lam = inp("lru_lam", [128, 2, 4])
    yT = k.dram("yT", [3, 512, T], F32, "ExternalOutput")
    uT = k.dram("uT_s", [D, T], BF16)
    pT = k.dram("pT_s", [NCH * 128, T], F32, kind="ExternalOutput" if dbg_p else "Internal")
    k.init_psum()
    ones, ident = consts(k)
    one_t = k.sb([128, 1], F32, "one_t"); k.memset(one_t, 1.0)
    mod = modulation(k, cT, ada_w, ada_b_l, 32, 0)
    nm = k.sb([128, KC], F32, "nm"); k.dma(nm[:], nmix, w=[nm])
    A_mix = k.sb([128, KC, 2], F32, "A_mix")
    k.ts(A_mix[:], mod[:, KC:2 * KC, :], 1.0, None, ALU.add, None, [mod], [A_mix])
    k.tt(A_mix[:], A_mix[:], nm[:].unsqueeze(2).to_broadcast([128, KC, 2]), ALU.mult, [A_mix, nm], [A_mix])

    def tview(ap2d, t0, nt):
        return ap2d[:, t0:t0 + nt].rearrange("(c p) t -> p c t", p=128)

    NTA = 512
    with contextlib.ExitStack() as st:
        Hs = [k.sb([128, KC, NTA], F32, "H", st) for _ in range(2)]
        tmp32 = k.sb([128, KC, NTA], F32, "tmp32", st)
        Us = [k.sb([128, KC, NTA], BF16, "U", st) for _ in range(2)]
        ssum = k.sb([128, NTA], F32, "ssum", st); rstd = k.sb([128, NTA], F32, "rstd", st)
        for i, (t0, nt, j) in enumerate(segs_tiles(TC, TL, NTA)):
            H = Hs[i % 2]; U = Us[i % 2]
            k.dma(H[:, :, 0:nt], tview(hT, t0, nt), w=[H], q="sp")
            norm_mod(k, H, nt, ones, A_mix, mod, j, U, tmp32, ssum, rstd, sh_off=0)
            k.dma(tview(uT, t0, nt), U[:, :, 0:nt], r=[U], q="pool")
        k.barrier()
    with contextlib.ExitStack() as st:
        GW = 10
        groups = [list(range(g, min(g + GW, NCH))) for g in range(0, NCH, GW)]
        wg = [k.sb([128, KC, GW * 128], BF16, "wg", st) for _ in range(2)]
        stg = [k.sb([128, KC, 128], F32, "stg", st) for _ in range(3)]
        Us = [k.sb([128, KC, NTA], BF16, "U2", st) for _ in range(2)]
        ob = [k.sb([128, NTA], F32, "ob", st) for _ in range(4)]
        si = 0; ui = 0; oi = 0
        for gi, grp in enumerate(groups):
            W = wg[gi % 2]
            for ci, ch in enumerate(grp):
                m = CH[ch][1]
                s_ = stg[si % 3]; si += 1
                k.dma(s_[:, :, 0:m], wview(w_sel, 0, KC, CHOFF[ch], m), w=[s_], q="sp" if si % 2 else "pool")
                k.copy(W[:, :, ci * 128:ci * 128 + m], s_[:, :, 0:m], [s_], [W], eng=("dve", "pool")[si % 2])
            for (t0, nt, j) in segs_tiles(TC, TL, NTA):
                U = Us[ui % 2]; ui += 1
                k.dma(U[:, :, 0:nt], tview(uT, t0, nt), w=[U], q="sp")
                for ci, ch in enumerate(grp):
                    m = CH[ch][1]
                    ps = k.ps()
                    for c in range(KC):
                        k.mm(ps[0:m, 0:nt], W[:, c, ci * 128:ci * 128 + m], U[:, c, 0:nt], [W, U], [ps], start=(c == 0), stop=(c == KC - 1))
                    o = ob[oi % 4]; oi += 1
                    k.copy(o[0:m, 0:nt], ps[0:m, 0:nt], [ps], [o], eng=("act", "dve")[oi % 2])
                    k.dma(pT[ch * 128:ch * 128 + m, t0:t0 + nt], o[0:m, 0:nt], r=[o], q="pool")
        k.barrier()

    segs = [(0, TC), (TC, T)]
    TT = 1024
    if "lru" in do:
        with contextlib.ExitStack() as st:
            cw = k.sb([128, 4, 4], F32, "lcw", st); cb = k.sb([128, 4], F32, "lcb", st)
            gb = k.sb([128, 2, 2, 4], F32, "lgb", st); lam = k.sb([128, 2, 4], F32, "llam", st); cs = k.sb([128, 2, 4], F32, "lcs", st)
            cs2 = k.sb([128, 2, 4], F32, "lcs2", st)
            gw = k.sb([128, 2, 2, 4, 128], F32, "lgw", st)
            k.dma(cw[:], lru_cw, w=[cw]); k.dma(cb[:], lru_cb, w=[cb]); k.dma(gb[:], lru_gb, w=[gb]); k.dma(lam[:], lru_lam, w=[lam])
            k.dma(gw[:].rearrange("p d g n j -> p (d g n) j"), lru_gw.rearrange("d g n k j -> k (d g n) j"), w=[gw])
            k.act(cs[:], lam[:], AF.Exp, [lam], [cs], scale=-1.0)
            k.act(cs[:], cs[:], AF.Ln, [cs], [cs], bias=one_t[:, 0:1])
            k.ts(cs2[:], cs[:], -16.0, None, ALU.mult, None, [cs], [cs2])
            k.ts(cs[:], cs[:], -8.0, None, ALU.mult, None, [cs], [cs])
            xc = k.sb([128, T], F32, "xc", st); hs = k.sb([128, T], F32, "hs", st)
            rg = k.sb([128, TT], F32, "rg", st); ig = k.sb([128, TT], F32, "ig", st); a_t = k.sb([128, TT], F32, "a_t", st)
            bx = k.sb([128, TT], F32, "bx", st); hb = [k.sb([128, TT], F32, "hb", st) for _ in range(2)]
            tiles = segs_tiles(TC, TL, TT)
            ctx_tiles = [t for t in tiles if t[2] == 1]; lat_tiles = [t for t in tiles if t[2] == 0]
            for blk in range(4):
                k.dma(hs[:], pT[CHI["lx0"] * 128 + blk * 128: CHI["lx0"] * 128 + (blk + 1) * 128, :], w=[hs])
                conv_seg(k, xc, hs, (cw, cw[:, blk, :], 0), (cb, cb, blk), segs)
                for d in range(2):
                    order = (ctx_tiles + lat_tiles) if d == 0 else (ctx_tiles[::-1] + lat_tiles[::-1])
                    prev = None
                    for ti, (s, n, isc) in enumerate(order):
                        for g, dst in ((0, rg), (1, ig)):
                            for c0 in range(0, n, 512):
                                cn = min(512, n - c0)
                                ps = k.ps()
                                k.mm(ps[:, 0:cn], gw[:, d, g, blk, :], xc[:, s + c0:s + c0 + cn], [gw, xc], [ps])
                                k.act(dst[:, c0:c0 + cn], ps[:, 0:cn], AF.Sigmoid, [ps, gb], [dst], bias=gb[:, d, g, blk:blk + 1])
                        k.act(a_t[:, 0:n], rg[:, 0:n], AF.Exp, [rg, cs], [a_t], scale=cs[:, d, blk:blk + 1])
                        k.act(rg[:, 0:n], rg[:, 0:n], AF.Exp, [rg, cs2], [rg], scale=cs2[:, d, blk:blk + 1])
                        k.ts(rg[:, 0:n], rg[:, 0:n], -1.0, 1.0, ALU.mult, ALU.add, [rg], [rg])
                        k.act(rg[:, 0:n], rg[:, 0:n], AF.Sqrt, [rg], [rg])
                        k.tt(bx[:, 0:n], rg[:, 0:n], ig[:, 0:n], ALU.mult, [rg, ig], [bx])
                        k.tt(bx[:, 0:n], bx[:, 0:n], xc[:, s:s + n], ALU.mult, [bx, xc], [bx])
                        if d == 0:
                            init = 0.0 if prev is None else prev
                            k.I("dve", "tensor_tensor_scan", [a_t, bx, hs], [hs], out=hs[:, s:s + n], data0=a_t[:, 0:n], data1=bx[:, 0:n],
                                initial=init, op0=ALU.mult, op1=ALU.add)
                            prev = hs[:, s + n - 1:s + n]
                        else:
                            hbt = hb[ti % 2]
                            init = 0.0 if prev is None else prev[0][:, 0:1]
                            rd = [a_t, bx] + ([prev[1]] if prev is not None else [])
                            k.I("dve", "tensor_tensor_scan", rd, [hbt], out=hbt[:, n - 1::-1], data0=a_t[:, n - 1::-1], data1=bx[:, n - 1::-1],
                                initial=init, op0=ALU.mult, op1=ALU.add)
                            prev = (hbt, hbt)
                            k.tt(hs[:, s:s + n], hs[:, s:s + n], hbt[:, 0:n], ALU.add, [hs, hbt], [hs], eng="pool")
                for (s, n, isc) in tiles:
                    k.dma(rg[:, 0:n], pT[CHI["lg0"] * 128 + blk * 128:CHI["lg0"] * 128 + (blk + 1) * 128, s:s + n], w=[rg])
                    k.tt(ig[:, 0:n], rg[:, 0:n], rg[:, 0:n], ALU.mult, [rg], [ig])
                    k.ts(ig[:, 0:n], ig[:, 0:n], 0.044715, 1.0, ALU.mult, ALU.add, [ig], [ig])
                    k.tt(ig[:, 0:n], ig[:, 0:n], rg[:, 0:n], ALU.mult, [ig, rg], [ig])
                    k.act(ig[:, 0:n], ig[:, 0:n], AF.Sigmoid, [ig], [ig], scale=1.5957691216057308)
                    k.tt(ig[:, 0:n], ig[:, 0:n], rg[:, 0:n], ALU.mult, [ig, rg], [ig])
                    k.tt(bx[:, 0:n], ig[:, 0:n], hs[:, s:s + n], ALU.mult, [ig, hs], [bx])
                    k.dma(yT[0, blk * 128:(blk + 1) * 128, s:s + n], bx[:, 0:n], r=[bx])
            k.barrier()
    if "ssd" in do:
        prm = dict(ssm_cw=inp("ssm_cw", [128, 6, 4]), ssm_cb=inp("ssm_cb", [128, 6]), ssm_A=inp("ssm_A", [8, 2]), ssm_dtb=inp("ssm_dtb", [8, 2]),
                   ssm_dsk=inp("ssm_dsk", [1, 8]), ssm_nw=inp("ssm_nw", [1, 512]))
        ssd_phase(k, TC, TL, pT, yT, prm, ident, ones, one_t)
    if "rwkv" in do:
        prm = dict(rw_mu=inp("rw_mu", [128, 18]), rw_w0=inp("rw_w0", [128, 2, 4]), rw_a0=inp("rw_a0", [128, 2, 4]), rw_kk=inp("rw_kk", [128, 4]),
                   rw_ka=inp("rw_ka", [128, 4]), rw_rk=inp("rw_rk", [128, 4]), rw_wup=inp("rw_wup", [2, 96, 512]), rw_aup=inp("rw_aup", [2, 96, 512]),
                   rw_gup=inp("rw_gup", [256, 512]), rw_lnw=inp("rw_lnw", [128, 4]), rw_lnb=inp("rw_lnb", [128, 4]))
        rwkv_phase(k, TC, TL, pT, yT, prm, ident, ones, one_t)
    k.finish()
    return nc, k


from concourse.bass_utils import run_bass_kernel_spmd

SEQ_, CTX_, GRID_W_ = 8192, 256, 64
OFF_LRU_, OFF_RWKV_, OFF_SSM_ = 6144, 10240, 17024
D_XBC_ = 3072
_PROG = {}


def _col_sel(q):
    cols = []
    O = OFF_LRU_
    cols += list(range(O + 512 * q, O + 512 * q + 512)); cols += list(range(O + 2048 + 512 * q, O + 2048 + 512 * q + 512))
    O = OFF_RWKV_
    for j in range(3):
        cols += list(range(O + 2048 * j + 512 * q, O + 2048 * j + 512 * q + 512))
    cols += list(range(O + 6144, O + 6144 + 640))
    O = OFF_SSM_
    cols += list(range(O + 512 * q, O + 512 * q + 512)); cols += list(range(O + 2048 + 512 * q, O + 2048 + 512 * q + 512))
    cols += list(range(O + 4096 + 128 * q, O + 4096 + 128 * q + 128)); cols += list(range(O + 4096 + 512 + 128 * q, O + 4096 + 512 + 128 * q + 128))
    O2 = O + 2048 + D_XBC_
    cols += list(range(O2 + 8 * q, O2 + 8 * q + 8)); cols += list(range(O2 + 32 + 8 * q, O2 + 32 + 8 * q + 8))
    return np.array(cols)


def _fm(v):
    return np.ascontiguousarray(np.asarray(v, np.float32).reshape(-1, 128).T)


def _stageA_params(P, li, q):
    sl = slice(512 * q, 512 * q + 512)
    hs_ = slice(8 * q, 8 * q + 8)
    ca = np.ascontiguousarray
    d = dict(
        ada_w=ca(P["ada_w"][li][:, :4096]), ada_b_l=_fm(P["ada_b"][li][:4096]), nmix=_fm(P["norm_mix"][li]),
        w_sel=ca(P["w_in"][li][:, _col_sel(q)]),
        lru_cw=ca(P["lru_conv_w"][li][:, sl].reshape(4, 4, 128).transpose(2, 1, 0)),
        lru_cb=_fm(P["lru_conv_b"][li][sl]), lru_gw=ca(P["lru_gate_w"][li][:, :, 4 * q:4 * q + 4]),
        lru_gb=ca(P["lru_gate_b"][li][:, :, sl].reshape(2, 2, 4, 128).transpose(3, 0, 1, 2)),
        lru_lam=ca(P["lru_lambda"][li][:, sl].reshape(2, 4, 128).transpose(2, 0, 1)))
    xbc_cols = np.concatenate([np.arange(512 * q, 512 * q + 512), 2048 + np.arange(128 * q, 128 * q + 128),
                               2048 + 512 + np.arange(128 * q, 128 * q + 128)])
    d.update(ssm_cw=ca(P["ssm_conv_w"][li][:, xbc_cols].reshape(4, 6, 128).transpose(2, 1, 0)),
             ssm_cb=_fm(P["ssm_conv_b"][li][xbc_cols]), ssm_A=ca(P["ssm_a_log"][li][:, hs_].T),
             ssm_dtb=ca(P["ssm_dt_bias"][li][:, hs_].T), ssm_dsk=ca(P["ssm_d"][li][hs_][None]), ssm_nw=ca(P["ssm_norm_w"][li][sl][None]))
    mu = P["rwkv_mu"][li]
    pad = lambda v: np.concatenate([v, np.zeros(128 - len(v), np.float32)])
    mucols = [mu[j * 2048 + 512 * q + i * 128: j * 2048 + 512 * q + (i + 1) * 128] for j in range(3) for i in range(4)]
    mucols += [pad(mu[6144 + i * 96:6144 + (i + 1) * 96]) for i in range(4)] + [mu[6144 + 384:6144 + 512], mu[6144 + 512:6144 + 640]]
    f4 = lambda v: ca(v[sl].reshape(4, 128).T)
    d.update(rw_mu=ca(np.stack(mucols, 1)), rw_w0=ca(P["rwkv_w0"][li][:, sl].reshape(2, 4, 128).transpose(2, 0, 1)),
             rw_a0=ca(P["rwkv_a0"][li][:, sl].reshape(2, 4, 128).transpose(2, 0, 1)), rw_kk=f4(P["rwkv_k_k"][li]), rw_ka=f4(P["rwkv_k_a"][li]),
             rw_rk=f4(P["rwkv_r_k"][li].reshape(-1)), rw_wup=ca(P["rwkv_w_up"][li][:, :, sl]), rw_aup=ca(P["rwkv_a_up"][li][:, :, sl]),
             rw_gup=ca(P["rwkv_g_up"][li][:, sl]), rw_lnw=f4(P["rwkv_ln_w"][li]), rw_lnb=f4(P["rwkv_ln_b"][li]))
    return {k_: np.asarray(v, np.float32) for k_, v in d.items()}


def _perm(a, rows, cols):
    return a.reshape(rows, cols, *a.shape[1:]).swapaxes(0, 1).reshape(a.shape)


def kernel(**inputs):
    P = {k_: np.asarray(v, np.float32) for k_, v in inputs.items()}
    Bn, TL, Dm = P["x"].shape
    TC = P["ctx"].shape[1]
    T = TC + TL
    rows = TL // GRID_W_
    depth = P["w_in"].shape[0]
    h_lat = P["x"].copy(); h_ctx = P["ctx"].copy()
    ncore = 8
    QN = ncore // Bn
    NLs = TL // QN; NCs = TC // QN
    if "A" not in _PROG:
        _PROG["A"] = build_A(TC, TL)[0]
    for li in range(depth):
        last = li == depth - 1
        odd = li % 2 == 1
        in_maps = []
        for b in range(Bn):
            lat = _perm(h_lat[b], rows, GRID_W_) if odd else h_lat[b]
            hT = np.ascontiguousarray(np.concatenate([h_ctx[b], lat], 0).T)
            cT = np.ascontiguousarray(np.stack([P["c"][b], P["c_ctx"]], 1))
            for q in range(QN):
                m = _stageA_params(P, li, q)
                m.update(hT=hT, cT=cT)
                in_maps.append(m)
        res = run_bass_kernel_spmd(_PROG["A"], in_maps, core_ids=list(range(ncore)))
        y_lat = np.empty((Bn, 3, TL, Dm), np.float32); y_ctx = np.empty((Bn, 3, TC, Dm), np.float32)
        for b in range(Bn):
            for q in range(QN):
                yT = np.asarray(res.results[b * QN + q]["yT"])
                for br in range(3):
                    yy = yT[br].T
                    y_ctx[b, br, :, 512 * q:512 * q + 512] = yy[:TC]
                    latp = yy[TC:]
                    y_lat[b, br, :, 512 * q:512 * q + 512] = _perm(latp, GRID_W_, rows) if odd else latp
        del res
        ncx = 0 if last else NCs
        kind = "dense" if li % 2 == 0 else "moe"
        key = ("B", kind, ncx, last)
        if key not in _PROG:
            _PROG[key] = build_B(NLs, ncx, kind, last)
        j = li // 2
        common = dict(ada_w=P["ada_w"][li], ada_b_l=_fm(P["ada_b"][li]), nmix=_fm(P["norm_mix"][li]), nffn=_fm(P["norm_ffn"][li]),
                      nfin=_fm(P["norm_final"]), w_gate=np.ascontiguousarray(P["w_in"][li][:, :6144]),
                      w_outs=np.ascontiguousarray(np.stack([P["w_out_lru"][li], P["w_out_rwkv"][li], P["w_out_ssm"][li]], 0)), w_o=P["w_o"][li])
        if kind == "dense":
            common.update(w1=P["ffn_w1"][j], w3=P["ffn_w3"][j], w2=P["ffn_w2"][j])
        else:
            common.update(router=P["moe_router"][j], mw1=P["moe_w1"][j], mw3=P["moe_w3"][j], mw2=P["moe_w2"][j])
        in_maps = []
        for b in range(Bn):
            cT = np.ascontiguousarray(np.stack([P["c"][b], P["c_ctx"]], 1))
            for s in range(QN):
                ls = slice(NLs * s, NLs * (s + 1)); cs = slice(ncx * s, ncx * (s + 1))
                hh = np.concatenate([h_lat[b, ls], h_ctx[b, cs]], 0) if ncx else h_lat[b, ls]
                yy = np.concatenate([y_lat[b, :, ls], y_ctx[b, :, cs]], 1) if ncx else y_lat[b, :, ls]
                m = dict(common)
                m.update(hT=np.ascontiguousarray(hh.T), yT=np.ascontiguousarray(yy.transpose(0, 2, 1)), cT=cT)
                in_maps.append(m)
        res = run_bass_kernel_spmd(_PROG[key], in_maps, core_ids=list(range(ncore)))
        for b in range(Bn):
            for s in range(QN):
                o = np.asarray(res.results[b * QN + s]["outT"]).T
                h_lat[b, NLs * s:NLs * (s + 1)] = o[:NLs]
                if ncx:
                    h_ctx[b, ncx * s:ncx * (s + 1)] = o[NLs:]
        del res
    return h_lat
```

```python
import contextlib
import numpy as np
import concourse.bass as bass
import concourse.mybir as mybir

F32 = mybir.dt.float32
BF16 = mybir.dt.bfloat16
AF = mybir.ActivationFunctionType
ALU = mybir.AluOpType
AX = mybir.AxisListType


class Tl:
    __slots__ = ("t", "w", "r", "name")

    def __init__(self, t, name):
        self.t = t
        self.w = None
        self.r = {}
        self.name = name

    def __getitem__(self, idx):
        return self.t[idx]


class Sub:
    def __init__(self, parent, ap, name):
        self.parent = parent
        self.t = ap
        self.name = name

    def __getitem__(self, idx):
        return self.t[idx]

    @property
    def w(self):
        return self.parent.w

    @w.setter
    def w(self, v):
        self.parent.w = v

    @property
    def r(self):
        return self.parent.r

    @r.setter
    def r(self, v):
        self.parent.r = v


class KB:
    NRING = 24

    def __init__(self, nc):
        self.nc = nc
        self.E = {"pe": nc.tensor, "act": nc.scalar, "dve": nc.vector, "pool": nc.gpsimd, "sp": nc.sync}
        self.es = contextlib.ExitStack()
        self.sem = {}
        self.cnt = {}
        for e in ("pe", "act", "dve", "pool"):
            self.sem[e] = self.es.enter_context(nc.semaphore("s_" + e))
            self.cnt[e] = 0
        for i in range(self.NRING):
            self.sem[("d", i)] = self.es.enter_context(nc.semaphore("s_d%d" % i))
            self.cnt[("d", i)] = 0
        self.ring = 0
        self.seen = {e: {} for e in self.E}
        self.ps_tiles = []
        self.ps_i = 0
        self.uid = 0
        self.nwait = 0

    def sb(self, shape, dt=F32, name=None, stack=None):
        self.uid += 1
        name = (name or "t") + "_%d" % self.uid
        t = (stack or self.es).enter_context(self.nc.sbuf_tensor(name, list(shape), dt))
        return Tl(t, name)

    def init_psum(self, n=8, cols=512):
        for i in range(n):
            t = self.es.enter_context(self.nc.psum_tensor("ps%d" % i, [128, cols], F32))
            self.ps_tiles.append(Tl(t, "ps%d" % i))

    def ps(self):
        t = self.ps_tiles[self.ps_i % len(self.ps_tiles)]
        self.ps_i += 1
        return t

    def dram(self, name, shape, dt=F32, kind="Internal"):
        return self.nc.dram_tensor(name, list(shape), dt, kind=kind).ap()

    def _need(self, eng, key, val, war=False):
        if key == eng and eng == "pe":
            return
        if self.seen[eng].get(key, 0) >= val:
            return
        self.E[eng].wait_ge(self.sem[key], val)
        self.nwait += 1
        self.seen[eng][key] = val

    def _sync(self, eng, r, w):
        for t in r:
            if t.w is not None:
                self._need(eng, *t.w)
        for t in w:
            if t.w is not None:
                self._need(eng, *t.w)
            for key, val in t.r.items():
                self._need(eng, key, val, war=True)

    def _post(self, ev, r, w):
        for t in w:
            t.w = ev
            t.r = {}
        for t in r:
            if t.r.get(ev[0], 0) < ev[1]:
                t.r[ev[0]] = ev[1]

    def I(self, eng, meth, r, w, *args, **kw):
        self._sync(eng, r, w)
        ins = getattr(self.E[eng], meth)(*args, **kw)
        self.cnt[eng] += 1
        ins.then_inc(self.sem[eng], 1)
        self._post((eng, self.cnt[eng]), r, w)
        return ins

    def dma(self, out, in_, r=(), w=(), q="sp", **kw):
        if q == "pool":
            q = "sp"
        key = ("d", self.ring % self.NRING)
        self.ring += 1
        self._sync(q, r, w)
        self._need(q, key, self.cnt[key])
        ins = self.E[q].dma_start(out=out, in_=in_, **kw)
        self.cnt[key] += 16
        ins.then_inc(self.sem[key], 16)
        self._post((key, self.cnt[key]), r, w)

    def barrier(self, engines=("pe", "act", "dve", "pool", "sp")):
        for e in engines:
            for key, c in self.cnt.items():
                if c > 0:
                    self._need(e, key, c)

    def finish(self):
        self.barrier()
        self.es.close()

    def mm(self, ps_ap, lhsT, rhs, r, w, start=True, stop=True):
        return self.I("pe", "matmul", r, w, ps_ap, lhsT=lhsT, rhs=rhs, start=start, stop=stop)

    def tr(self, ps_ap, in_ap, ident_ap, r, w):
        return self.I("pe", "transpose", r, w, ps_ap, in_ap, ident_ap)

    def act(self, out, in_, func, r, w, **kw):
        return self.I("act", "activation", r, w, out=out, in_=in_, func=func, **kw)

    def tt(self, out, in0, in1, op, r, w, eng="dve"):
        return self.I(eng, "tensor_tensor", r, w, out=out, in0=in0, in1=in1, op=op)

    def ts(self, out, in0, s1, s2, op0, op1, r, w, eng="dve"):
        if op1 is None:
            return self.I(eng, "tensor_scalar", r, w, out=out, in0=in0, scalar1=s1, scalar2=None, op0=op0)
        return self.I(eng, "tensor_scalar", r, w, out=out, in0=in0, scalar1=s1, scalar2=s2, op0=op0, op1=op1)

    def stt(self, out, in0, scalar, in1, op0, op1, r, w):
        return self.I("dve", "scalar_tensor_tensor", r, w, out=out, in0=in0, scalar=scalar, in1=in1, op0=op0, op1=op1)

    def copy(self, out, in_, r, w, eng="dve"):
        if eng == "act":
            return self.I("act", "copy", r, w, out=out, in_=in_)
        return self.I(eng, "tensor_copy", r, w, out=out, in_=in_)

    def memset(self, t, val, eng="pool"):
        return self.I(eng, "memset", [], [t], t[:], val)


D = 2048
KC = 16


class WS:
    def __init__(self, k, nstg=3, nbf=2, look=2, cast_engs=("dve", "pool", "act")):
        self.k = k
        self.stg = [k.sb([128, KC, 128], F32, "wstg") for _ in range(nstg)]
        self.bf = [k.sb([128, KC, 128], BF16, "wbf") for _ in range(nbf)]
        self.look = look
        self.cast_engs = cast_engs
        self.pieces = []
        self.n_dma = 0
        self.n_cast = 0

    def set(self, pieces):
        self.pieces = pieces
        self.n_dma = 0
        self.n_cast = 0

    def _dma(self):
        i = self.n_dma
        if i >= len(self.pieces):
            return
        view, kc, m = self.pieces[i]
        st = self.stg[i % len(self.stg)]
        q = "sp" if i % 2 == 0 else "pool"
        self.k.dma(st[:, 0:kc, 0:m], view, w=[st], q=q)
        self.n_dma += 1

    def _cast(self):
        i = self.n_cast
        if i >= len(self.pieces):
            return
        view, kc, m = self.pieces[i]
        st = self.stg[i % len(self.stg)]
        bf = self.bf[i % len(self.bf)]
        eng = self.cast_engs[i % len(self.cast_engs)]
        self.k.copy(bf[:, 0:kc, 0:m], st[:, 0:kc, 0:m], [st], [bf], eng=eng)
        self.n_cast += 1

    def take(self, i):
        while self.n_dma < min(len(self.pieces), i + 1 + self.look):
            self._dma()
        while self.n_cast < min(len(self.pieces), i + 2):
            self._cast()
        return self.bf[i % len(self.bf)]


def wview(w, k0, kc, c0, m):
    return w[k0 * 128:(k0 + kc) * 128, c0:c0 + m].rearrange("(c p) n -> p c n", p=128)


def run_jobs(k, ws, jobs):
    pieces = []
    for jb in jobs:
        jb["p0"] = len(pieces)
        pieces.extend(jb["pieces"])
    ws.set(pieces)
    for jb in jobs:
        if jb.get("pre"):
            jb["pre"]()
        ps = k.ps()
        x = jb["x"]
        nt = jb["nt"]
        np_ = len(jb["pieces"])
        kbase = 0
        for pi, (view, kc, m) in enumerate(jb["pieces"]):
            bf = ws.take(jb["p0"] + pi)
            for c in range(kc):
                k.mm(ps[0:m, 0:nt], bf[:, c, 0:m], x[:, kbase + c, 0:nt], [bf, x], [ps],
                     start=(pi == 0 and c == 0), stop=(pi == np_ - 1 and c == kc - 1))
            kbase += kc
        jb["epi"](ps)


def modulation(k, cT, ada_w, ada_b_l, ncols_chunks, col_chunk0, ident=None):
    mod = k.sb([128, ncols_chunks, 2], F32, "mod")
    with contextlib.ExitStack() as st:
        c_t = k.sb([128, KC, 2], F32, "c_t", st)
        sc_t = k.sb([128, KC, 2], F32, "sc_t", st)
        k.dma(c_t[:], cT.rearrange("(c p) j -> p c j", p=128), w=[c_t])
        k.act(sc_t[:], c_t[:], AF.Silu, [c_t], [sc_t])
        bl = k.sb([128, ncols_chunks], F32, "adab", st)
        k.dma(bl[:], ada_b_l[:, col_chunk0:col_chunk0 + ncols_chunks], w=[bl])
        wbuf = [k.sb([128, KC, 512], F32, "adaw", st) for _ in range(2)]
        ngrp = (ncols_chunks + 3) // 4
        for g in range(ngrp):
            wb = wbuf[g % 2]
            ncc = min(4, ncols_chunks - g * 4)
            c0 = (col_chunk0 + g * 4) * 128
            k.dma(wb[:, :, 0:ncc * 128], wview(ada_w, 0, KC, c0, ncc * 128), w=[wb], q="sp" if g % 2 == 0 else "pool")
            for cc in range(ncc):
                ps = k.ps()
                for c in range(KC):
                    k.mm(ps[:, 0:2], wb[:, c, cc * 128:(cc + 1) * 128], sc_t[:, c, :], [wb, sc_t], [ps],
                         start=(c == 0), stop=(c == KC - 1))
                k.tt(mod[:, g * 4 + cc, :], ps[:, 0:2], bl[:, g * 4 + cc:g * 4 + cc + 1].to_broadcast([128, 2]), ALU.add,
                     [ps, bl], [mod])
        k.barrier(("sp", "pool", "pe", "dve", "act"))
    return mod


def norm_mod(k, H, nt, ones, A, sh, j, out_bf, tmp32, ssum, rstd, out32=None, sh_off=0):
    k.tt(tmp32[:, :, 0:nt], H[:, :, 0:nt], H[:, :, 0:nt], ALU.mult, [H], [tmp32])
    k.I("dve", "tensor_reduce", [tmp32], [ssum], out=ssum[:, 0:nt], in_=tmp32[:, :, 0:nt].rearrange("p c t -> p t c"),
        axis=AX.X, op=ALU.add)
    ps = k.ps()
    k.mm(ps[:, 0:nt], ones[:, :], ssum[:, 0:nt], [ones, ssum], [ps])
    k.act(rstd[:, 0:nt], ps[:, 0:nt], AF.Sqrt, [ps], [rstd], scale=1.0 / D, bias=k.eps_t[:, 0:1])
    k.I("dve", "reciprocal", [rstd], [rstd], out=rstd[:, 0:nt], in_=rstd[:, 0:nt])
    k.tt(tmp32[:, :, 0:nt], H[:, :, 0:nt], rstd[:, 0:nt].unsqueeze(1).to_broadcast([128, KC, nt]), ALU.mult,
         [H, rstd], [tmp32])
    for c in range(KC):
        for o in (out_bf, out32):
            if o is None:
                continue
            if sh is not None:
                k.ts(o[:, c, 0:nt], tmp32[:, c, 0:nt], A[:, c, j:j + 1], sh[:, sh_off + c, j:j + 1], ALU.mult, ALU.add,
                     [tmp32, A, sh], [o])
            else:
                k.ts(o[:, c, 0:nt], tmp32[:, c, 0:nt], A[:, c, j:j + 1], None, ALU.mult, None, [tmp32, A], [o])


def consts(k):
    k.eps_t = k.sb([128, 1], F32, "eps")
    k.memset(k.eps_t, 1e-6)
    ones = k.sb([128, 128], F32, "ones")
    k.memset(ones, 1.0)
    ident = k.sb([128, 128], F32, "ident")
    k.memset(ident, 1.0)
    k.I("pool", "affine_select", [ident], [ident], out=ident[:], in_=ident[:], pattern=[[-1, 128]],
        compare_op=ALU.is_equal, fill=0.0, base=0, channel_multiplier=1)
    return ones, ident


def build_B(NL, NCX, kind, final_norm, NT=512):
    NB = NL + NCX
    nc = bass.Bass("TRN2", target_bir_lowering=False)
    k = KB(nc)
    inp = lambda n, s: k.dram(n, s, F32, "ExternalInput")
    hT = inp("hT", [D, NB]); yT = inp("yT", [3, D, NB]); cT = inp("cT", [D, 2])
    ada_w = inp("ada_w", [D, 6 * D]); ada_b_l = inp("ada_b_l", [128, 96])
    nmix = inp("nmix", [128, KC]); nffn = inp("nffn", [128, KC]); nfin = inp("nfin", [128, KC])
    w_gate = inp("w_gate", [D, 3 * D]); w_outs = inp("w_outs", [3, D, D]); w_o = inp("w_o", [D, D])
    if kind == "dense":
        w1 = inp("w1", [D, 3 * D]); w3 = inp("w3", [D, 3 * D]); w2 = inp("w2", [3 * D, D])
    else:
        router = inp("router", [D, 8]); mw1 = inp("mw1", [8, D, 3072]); mw3 = inp("mw3", [8, D, 3072])
        mw2 = inp("mw2", [8, 3072, D])
    outT = k.dram("outT", [D, NB], F32, "ExternalOutput")

    k.init_psum()
    ones, ident = consts(k)
    mod = modulation(k, cT, ada_w, ada_b_l, 96, 0)
    nm = k.sb([128, KC], F32, "nm"); nf = k.sb([128, KC], F32, "nf"); nfi = k.sb([128, KC, 2], F32, "nfi")
    k.dma(nm[:], nmix, w=[nm]); k.dma(nf[:], nffn, w=[nf])
    nfi1 = k.sb([128, KC], F32, "nfi1"); k.dma(nfi1[:], nfin, w=[nfi1])
    k.copy(nfi[:], nfi1[:].unsqueeze(2).to_broadcast([128, KC, 2]), [nfi1], [nfi])
    A_mix = k.sb([128, KC, 2], F32, "A_mix"); A_ffn = k.sb([128, KC, 2], F32, "A_ffn")
    for (A, g, sec) in ((A_mix, nm, 1), (A_ffn, nf, 4)):
        k.ts(A[:], mod[:, sec * KC:(sec + 1) * KC, :], 1.0, None, ALU.add, None, [mod], [A])
        k.tt(A[:], A[:], g[:].unsqueeze(2).to_broadcast([128, KC, 2]), ALU.mult, [A, g], [A])
    shm = mod
    SHM, GM, SHF, GF = 0, 2, 3, 5

    H = k.sb([128, KC, NT], F32, "H"); U = k.sb([128, KC, NT], BF16, "U"); M = k.sb([128, KC, NT], F32, "M")
    big32 = k.sb([128, KC, NT], F32, "big32"); YB = k.sb([128, KC, NT], BF16, "YB")
    MB = YB
    nh = 24
    HID = k.sb([128, nh, NT], BF16, "HID")
    ssum = k.sb([128, NT], F32, "ssum"); rstd = k.sb([128, NT], F32, "rstd")
    gs = [k.sb([128, NT], F32, "gs") for _ in range(2)]
    tmpe = [k.sb([128, NT], F32, "tmpe") for _ in range(2)]
    ws = WS(k)
    if kind == "moe":
        rt = k.sb([128, KC, 8], F32, "rt")
        k.dma(rt[:], router.rearrange("(c p) e -> p c e", p=128), w=[rt])
        V32 = big32
        selE = k.sb([8, 8, 128], F32, "selE")
        k.memset(selE, 1.0)
        k.I("pool", "affine_select", [selE], [selE], out=selE[:], in_=selE[:], pattern=[[-1, 8], [0, 128]],
            compare_op=ALU.is_equal, fill=0.0, base=0, channel_multiplier=1)
        lg = tmpe[0]; nblk = (NT + 127) // 128
        ltok = k.sb([128, nblk, 8], F32, "ltok"); t8 = k.sb([128, nblk, 8], F32, "t8"); t8b = k.sb([128, nblk, 8], F32, "t8b")
        m1 = k.sb([128, nblk], F32, "m1"); m2 = k.sb([128, nblk], F32, "m2")
        gT = tmpe[1]; gbc = big32
        ACC = M

    OUT = M if final_norm else None
    tiles = [(t0, min(NT, NL - t0), 0) for t0 in range(0, NL, NT)]
    if NCX:
        tiles += [(NL + t0, min(NT, NCX - t0), 1) for t0 in range(0, NCX, NT)]

    def tview(ap2d, t0, nt):
        return ap2d[:, t0:t0 + nt].rearrange("(c p) t -> p c t", p=128)

    cnt = [0]
    for (t0, nt, j) in tiles:
        jobs = []

        def pre_tile(t0=t0, nt=nt, j=j):
            k.dma(H[:, :, 0:nt], tview(hT, t0, nt), w=[H])
            norm_mod(k, H, nt, ones, A_mix, mod, j, U, big32, ssum, rstd, sh_off=SHM * KC)

        first = [True]
        for br in range(3):
            def pre_br(br=br, t0=t0, nt=nt):
                k.dma(big32[:, :, 0:nt], tview(yT[br], t0, nt), w=[big32], q="pool")
                k.copy(YB[:, :, 0:nt], big32[:, :, 0:nt], [big32], [YB])

            for oc in range(KC):
                i = cnt[0]; cnt[0] += 1
                g_t = gs[i % 2]; te = tmpe[i % 2]

                def epi_g(ps, g_t=g_t, nt=nt):
                    k.act(g_t[:, 0:nt], ps[:, 0:nt], AF.Sigmoid, [ps], [g_t])

                def epi_y(ps, g_t=g_t, te=te, br=br, oc=oc, nt=nt):
                    if br == 0:
                        k.tt(M[:, oc, 0:nt], ps[:, 0:nt], g_t[:, 0:nt], ALU.mult, [ps, g_t], [M])
                    else:
                        k.tt(te[:, 0:nt], ps[:, 0:nt], g_t[:, 0:nt], ALU.mult, [ps, g_t], [te])
                        k.tt(M[:, oc, 0:nt], M[:, oc, 0:nt], te[:, 0:nt], ALU.add, [M, te], [M], eng="pool")

                pre = None
                if br == 0 and oc == 0:
                    def pre(pre_tile=pre_tile, pre_br=pre_br):
                        pre_tile(); pre_br()
                elif oc == 0:
                    pre = pre_br
                jobs.append(dict(pre=pre, pieces=[(wview(w_gate, 0, KC, br * D + oc * 128, 128), KC, 128)], x=U, nt=nt, epi=epi_g))
                jobs.append(dict(pieces=[(wview(w_outs[br], 0, KC, oc * 128, 128), KC, 128)], x=YB, nt=nt, epi=epi_y))

        def pre_o(nt=nt):
            k.copy(MB[:, :, 0:nt], M[:, :, 0:nt], [M], [MB])

        for oc in range(KC):
            def epi_o(ps, oc=oc, nt=nt, j=j):
                k.stt(H[:, oc, 0:nt], ps[:, 0:nt], mod[:, GM * KC + oc, j:j + 1], H[:, oc, 0:nt], ALU.mult, ALU.add,
                      [ps, mod, H], [H])
            jobs.append(dict(pre=pre_o if oc == 0 else None, pieces=[(wview(w_o, 0, KC, oc * 128, 128), KC, 128)], x=MB, nt=nt, epi=epi_o))

        def pre_ffn(nt=nt, j=j):
            norm_mod(k, H, nt, ones, A_ffn, mod, j, U, big32, ssum, rstd, sh_off=SHF * KC)

        if kind == "dense":
            for half in range(2):
                for hc in range(24):
                    i = cnt[0]; cnt[0] += 1
                    sa = gs[i % 2]
                    col = (half * 24 + hc) * 128

                    def epi_a(ps, sa=sa, nt=nt):
                        k.act(sa[:, 0:nt], ps[:, 0:nt], AF.Silu, [ps], [sa])

                    def epi_b(ps, sa=sa, hc=hc, nt=nt):
                        k.tt(HID[:, hc, 0:nt], ps[:, 0:nt], sa[:, 0:nt], ALU.mult, [ps, sa], [HID])
                    jobs.append(dict(pre=pre_ffn if (hc == 0 and half == 0) else None, pieces=[(wview(w1, 0, KC, col, 128), KC, 128)], x=U, nt=nt, epi=epi_a))
                    jobs.append(dict(pieces=[(wview(w3, 0, KC, col, 128), KC, 128)], x=U, nt=nt, epi=epi_b))
                for oc in range(KC):
                    def epi_f(ps, oc=oc, nt=nt, j=j, half=half):
                        if half == 0:
                            k.copy(M[:, oc, 0:nt], ps[:, 0:nt], [ps], [M], eng="act")
                        else:
                            k.tt(M[:, oc, 0:nt], M[:, oc, 0:nt], ps[:, 0:nt], ALU.add, [M, ps], [M])
                            k.stt(H[:, oc, 0:nt], M[:, oc, 0:nt], mod[:, GF * KC + oc, j:j + 1], H[:, oc, 0:nt], ALU.mult, ALU.add,
                                  [M, mod, H], [H])
                    jobs.append(dict(pieces=[(wview(w2, half * 24, KC, oc * 128, 128), KC, 128), (wview(w2, half * 24 + KC, 8, oc * 128, 128), 8, 128)],
                                     x=HID, nt=nt, epi=epi_f))
        else:
            def pre_moe(nt=nt, j=j, t0=t0):
                norm_mod(k, H, nt, ones, A_ffn, mod, j, U, big32, ssum, rstd, sh_off=SHF * KC)
                for c in range(KC):
                    k.ts(big32[:, c, 0:nt], big32[:, c, 0:nt], A_ffn[:, c, j:j + 1], mod[:, SHF * KC + c, j:j + 1],
                         ALU.mult, ALU.add, [big32, A_ffn, mod], [big32])
                ps = k.ps()
                for c in range(KC):
                    k.mm(ps[0:8, 0:nt], rt[:, c, :], big32[:, c, 0:nt], [rt, big32], [ps], start=(c == 0), stop=(c == KC - 1))
                k.copy(lg[0:8, 0:nt], ps[0:8, 0:nt], [ps], [lg])
                nb = (nt + 127) // 128
                for b in range(nb):
                    w_ = min(128, nt - b * 128)
                    ps2 = k.ps()
                    k.tr(ps2[0:w_, 0:8], lg[0:8, b * 128:b * 128 + w_], ident[0:8, 0:8], [lg, ident], [ps2])
                    k.copy(ltok[0:w_, b, :], ps2[0:w_, 0:8], [ps2], [ltok])
                if nt % 128:
                    pass
                P = min(128, nt)
                bc = lambda t_: t_[0:P, 0:nb].unsqueeze(2).to_broadcast([P, nb, 8])
                k.I("dve", "tensor_reduce", [ltok], [m1], out=m1[0:P, 0:nb], in_=ltok[0:P, 0:nb, :], axis=AX.X, op=ALU.max)
                k.tt(t8[0:P, 0:nb, :], ltok[0:P, 0:nb, :], bc(m1), ALU.is_equal, [ltok, m1], [t8])
                k.ts(t8[0:P, 0:nb, :], t8[0:P, 0:nb, :], -1e30, None, ALU.mult, None, [t8], [t8])
                k.tt(t8[0:P, 0:nb, :], t8[0:P, 0:nb, :], ltok[0:P, 0:nb, :], ALU.add, [t8, ltok], [t8])
                k.I("dve", "tensor_reduce", [t8], [m2], out=m2[0:P, 0:nb], in_=t8[0:P, 0:nb, :], axis=AX.X, op=ALU.max)
                k.tt(t8[0:P, 0:nb, :], ltok[0:P, 0:nb, :], bc(m2), ALU.is_ge, [ltok, m2], [t8])
                k.tt(t8b[0:P, 0:nb, :], ltok[0:P, 0:nb, :], bc(m1), ALU.subtract, [ltok, m1], [t8b])
                k.act(t8b[0:P, 0:nb, :], t8b[0:P, 0:nb, :], AF.Exp, [t8b], [t8b])
                k.tt(t8b[0:P, 0:nb, :], t8b[0:P, 0:nb, :], t8[0:P, 0:nb, :], ALU.mult, [t8b, t8], [t8b])
                k.I("dve", "tensor_reduce", [t8b], [m2], out=m2[0:P, 0:nb], in_=t8b[0:P, 0:nb, :], axis=AX.X, op=ALU.add)
                k.I("dve", "reciprocal", [m2], [m2], out=m2[0:P, 0:nb], in_=m2[0:P, 0:nb])
                k.tt(t8b[0:P, 0:nb, :], t8b[0:P, 0:nb, :], bc(m2), ALU.mult, [t8b, m2], [t8b])
                for b in range(nb):
                    w_ = min(128, nt - b * 128)
                    ps2 = k.ps()
                    k.tr(ps2[0:8, 0:w_], t8b[0:w_, b, :], ident[0:w_, 0:w_], [t8b, ident], [ps2])
                    k.copy(gT[0:8, b * 128:b * 128 + w_], ps2[0:8, 0:w_], [ps2], [gT])
                for e in range(8):
                    ps2 = k.ps()
                    k.mm(ps2[:, 0:nt], selE[0:8, e, :], gT[0:8, 0:nt], [selE, gT], [ps2])
                    k.copy(gbc[:, e, 0:nt], ps2[:, 0:nt], [ps2], [gbc], eng="act")

            for e in range(8):
                for hc in range(24):
                    i = cnt[0]; cnt[0] += 1
                    sa = gs[i % 2]

                    def epi_a(ps, sa=sa, nt=nt, e=e):
                        k.act(sa[:, 0:nt], ps[:, 0:nt], AF.Silu, [ps], [sa])
                        k.tt(sa[:, 0:nt], sa[:, 0:nt], gbc[:, e, 0:nt], ALU.mult, [sa, gbc], [sa], eng="pool")

                    def epi_b(ps, sa=sa, hc=hc, nt=nt):
                        k.tt(HID[:, hc, 0:nt], ps[:, 0:nt], sa[:, 0:nt], ALU.mult, [ps, sa], [HID])
                    jobs.append(dict(pre=pre_moe if (e == 0 and hc == 0) else None,
                                     pieces=[(wview(mw1[e], 0, KC, hc * 128, 128), KC, 128)], x=U, nt=nt, epi=epi_a))
                    jobs.append(dict(pieces=[(wview(mw3[e], 0, KC, hc * 128, 128), KC, 128)], x=U, nt=nt, epi=epi_b))
                for oc in range(KC):
                    def epi_f(ps, oc=oc, nt=nt, e=e):
                        if e == 0:
                            k.copy(ACC[:, oc, 0:nt], ps[:, 0:nt], [ps], [ACC], eng="act")
                        else:
                            k.tt(ACC[:, oc, 0:nt], ACC[:, oc, 0:nt], ps[:, 0:nt], ALU.add, [ACC, ps], [ACC])
                    jobs.append(dict(pieces=[(wview(mw2[e], 0, KC, oc * 128, 128), KC, 128), (wview(mw2[e], KC, 8, oc * 128, 128), 8, 128)],
                                     x=HID, nt=nt, epi=epi_f))
        run_jobs(k, ws, jobs)
        if kind == "moe":
            for oc in range(KC):
                k.stt(H[:, oc, 0:nt], ACC[:, oc, 0:nt], mod[:, GF * KC + oc, j:j + 1], H[:, oc, 0:nt], ALU.mult, ALU.add,
                      [ACC, mod, H], [H])
        if final_norm:
            norm_mod(k, H, nt, ones, nfi, None, 0, None, big32, ssum, rstd, out32=OUT)
            k.dma(tview(outT, t0, nt), OUT[:, :, 0:nt], r=[OUT])
        else:
            k.dma(tview(outT, t0, nt), H[:, :, 0:nt], r=[H])
    k.finish()
    return nc


def ssd_phase(k, TC, TL, pT, yT, prm, ident, ones, one_t):
    T = TC + TL
    xbc_s = k.dram("xbc_s", [6 * 128, T], F32)
    dti_s = k.dram("dti_s", [2, 2, 8, T], F32)
    yf_s = k.dram("yf_s", [T, 512], F32)
    segs = [(0, TC), (TC, T)]
    with contextlib.ExitStack() as st:
        cw = k.sb([128, 6, 4], F32, "scw", st); cb = k.sb([128, 6], F32, "scb", st)
        k.dma(cw[:], prm["ssm_cw"], w=[cw]); k.dma(cb[:], prm["ssm_cb"], w=[cb])
        pin = [k.sb([128, T], F32, "pin", st) for _ in range(2)]
        xo = [k.sb([128, T], F32, "xo", st) for _ in range(2)]
        for blk in range(6):
            ch = CHI["sx0"] + blk
            p_ = pin[blk % 2]; x_ = xo[blk % 2]
            k.dma(p_[:], pT[ch * 128:(ch + 1) * 128, :], w=[p_], q="sp")
            conv_seg(k, x_, p_, (cw, cw[:, blk, :], 0), (cb, cb, blk), segs)
            k.act(x_[:], x_[:], AF.Silu, [x_], [x_])
            k.dma(xbc_s[blk * 128:(blk + 1) * 128, :], x_[:], r=[x_], q="pool")
        k.barrier()
    with contextlib.ExitStack() as st:
        PW = min(T, 2048)
        al = k.sb([8, 2], F32, "al", st); dtb = k.sb([8, 2], F32, "dtb", st)
        k.dma(al[:], prm["ssm_A"], w=[al]); k.dma(dtb[:], prm["ssm_dtb"], w=[dtb])
        k.act(al[:], al[:], AF.Exp, [al], [al])
        k.ts(al[:], al[:], -1.0, None, ALU.mult, None, [al], [al])
        mf = k.sb([8, PW], F32, "mf", st); mb = k.sb([8, PW], F32, "mb", st)
        k.memset(mf, 1.0); k.memset(mb, 1.0)
        k.I("pool", "memset", [], [mf], mf[:, 0::128], 0.0)
        k.I("pool", "memset", [], [mb], mb[:, 127::128], 0.0)
        xx = k.sb([8, PW], F32, "dxx", st); dt_ = k.sb([8, PW], F32, "ddt", st); cum = k.sb([8, PW], F32, "dcum", st)
        for d in range(2):
            ch = CHI["dt0"] + d
            for p0 in range(0, T, PW):
                n = min(PW, T - p0)
                k.dma(xx[:, 0:n], pT[ch * 128:ch * 128 + 8, p0:p0 + n], w=[xx])
                k.act(xx[:, 0:n], xx[:, 0:n], AF.Exp, [xx, dtb], [xx], bias=dtb[:, d:d + 1])
                k.act(dt_[:, 0:n], xx[:, 0:n], AF.Ln, [xx], [dt_], bias=one_t[0:8, 0:1])
                k.ts(xx[:, 0:n], dt_[:, 0:n], al[:, d:d + 1], None, ALU.mult, None, [dt_, al], [xx])
                if d == 0:
                    k.I("dve", "tensor_tensor_scan", [mf, xx], [cum], out=cum[:, 0:n], data0=mf[:, 0:n], data1=xx[:, 0:n],
                        initial=0.0, op0=ALU.mult, op1=ALU.add)
                else:
                    k.I("dve", "tensor_tensor_scan", [mb, xx], [cum], out=cum[:, n - 1::-1], data0=mb[:, n - 1::-1], data1=xx[:, n - 1::-1],
                        initial=0.0, op0=ALU.mult, op1=ALU.add)
                k.act(dt_[:, 0:n], dt_[:, 0:n], AF.Ln, [dt_], [dt_])
                k.tt(dt_[:, 0:n], dt_[:, 0:n], cum[:, 0:n], ALU.subtract, [dt_, cum], [dt_])
                k.dma(dti_s[d, 0, :, p0:p0 + n], dt_[:, 0:n], r=[dt_]); k.dma(dti_s[d, 1, :, p0:p0 + n], cum[:, 0:n], r=[cum])
        k.barrier()
    with contextlib.ExitStack() as st:
        maskf = k.sb([128, 128], F32, "maskf", st); maskb = k.sb([128, 128], F32, "maskb", st)
        k.memset(maskf, 1.0); k.memset(maskb, 1.0)
        k.I("pool", "affine_select", [maskf], [maskf], out=maskf[:], in_=maskf[:], pattern=[[1, 128]], compare_op=ALU.is_ge, fill=0.0, base=0, channel_multiplier=-1)
        k.I("pool", "affine_select", [maskb], [maskb], out=maskb[:], in_=maskb[:], pattern=[[-1, 128]], compare_op=ALU.is_ge, fill=0.0, base=0, channel_multiplier=1)
        BM = k.sb([40, 8, 128], F32, "BM", st)
        k.memset(BM, 1.0)
        k.I("pool", "affine_select", [BM], [BM], out=BM[0:8], in_=BM[0:8], pattern=[[-1, 8], [0, 128]], compare_op=ALU.is_equal, fill=0.0, base=0, channel_multiplier=1)
        k.dma(BM[32:40], BM[0:8], r=[BM], w=[BM])
        Lt = [k.sb([40, 128], F32, "Lt", st) for _ in range(2)]; Ct = [k.sb([40, 128], F32, "Ct", st) for _ in range(2)]
        Rt = [k.sb([40, 8, 128], F32, "Rt", st) for _ in range(2)]
        for i in range(2):
            k.memset(Lt[i], 0.0); k.memset(Ct[i], 0.0); k.memset(Rt[i], 0.0)
            k.I("pool", "memset", [], [Lt[i]], Lt[i][32:40, :], 1.0)
            k.copy(Rt[i][0:8], BM[0:8], [BM], [Rt[i]], eng="pool")
        XBC = [k.sb([128, 6, 128], F32, "XBC", st) for _ in range(2)]
        xtok = k.sb([128, 512], F32, "xtok", st); btok = k.sb([128, 128], F32, "btok", st)
        CBm = k.sb([128, 128], F32, "CBm", st); E = k.sb([128, 8, 128], F32, "E", st); S = k.sb([128, 8, 128], F32, "S", st)
        xw = k.sb([128, 512], F32, "xw", st); ecum = k.sb([128, 8], F32, "ecum", st); dc = k.sb([128, 8], F32, "dc", st)
        dg = k.sb([40, 8], F32, "dg", st)
        ych = [k.sb([128, 512], F32, "ych", st) for _ in range(2)]; yo = k.sb([128, 512], F32, "yo", st)
        Hst = k.sb([128, 512], F32, "Hst", st)
        yfl = k.sb([128, 512], F32, "yfl", st); zT = k.sb([128, 4, 128], F32, "zT", st); yg = k.sb([128, 512], F32, "yg", st)
        junk = k.sb([128, 512], F32, "junk", st); ss = k.sb([128, 1], F32, "ss", st)
        nw = k.sb([128, 512], F32, "nw", st); dsk = k.sb([128, 8], F32, "dsk", st); yot = k.sb([128, 4, 128], F32, "yot", st)
        k.dma(nw[:], prm["ssm_nw"][0:1, :].partition_broadcast(128), w=[nw])
        k.dma(dsk[:], prm["ssm_dsk"][0:1, :].partition_broadcast(128), w=[dsk])
        cchunks = list(range(0, TC, 128)); lchunks = list(range(TC, T, 128))
        it = 0
        for d in range(2):
            order = (cchunks + lchunks) if d == 0 else (cchunks[::-1] + lchunks[::-1])
            mask = maskf if d == 0 else maskb
            last = 127 if d == 0 else 0
            k.memset(Hst, 0.0, eng="dve")
            for t0 in order:
                X = XBC[it % 2]; L = Lt[it % 2]; C = Ct[it % 2]; R = Rt[it % 2]; Y = ych[it % 2]; it += 1
                k.dma(X[:], xbc_s[:, t0:t0 + 128].rearrange("(b p) t -> p b t", p=128), w=[X], q="sp")
                k.dma(L[0:8, :], dti_s[d, 0, :, t0:t0 + 128], w=[L], q="pool")
                k.dma(C[32:40, :], dti_s[d, 1, :, t0:t0 + 128], w=[C], q="pool")
                pst = k.ps()
                for b in range(4):
                    k.tr(pst[:, b * 128:(b + 1) * 128], X[:, b, :], ident[:, :], [X, ident], [pst])
                k.copy(xtok[:], pst[:, :], [pst], [xtok], eng="act")
                psb = k.ps()
                k.tr(psb[:, 0:128], X[:, 4, :], ident[:, :], [X, ident], [psb])
                k.copy(btok[:], psb[:, 0:128], [psb], [btok], eng="act")
                pcb = k.ps()
                k.mm(pcb[:, 0:128], X[:, 4, :], X[:, 5, :], [X], [pcb])
                k.tt(CBm[:], pcb[:, 0:128], mask[:], ALU.mult, [pcb, mask], [CBm])
                k.tt(R[32:40], C[32:40, :].unsqueeze(1).to_broadcast([8, 8, 128]), BM[32:40], ALU.mult, [C, BM], [R])
                for hh in range(2):
                    psg = k.ps()
                    k.mm(psg[:, :], L[0:40, :], R[0:40, hh * 4:(hh + 1) * 4, :], [L, R], [psg])
                    k.ts(E[:, hh * 4:(hh + 1) * 4, :], psg[:, :].rearrange("p (a b) -> p a b", a=4), 30.0, None, ALU.min, None, [psg], [E])
                k.act(E[:], E[:], AF.Exp, [E], [E])
                k.tt(S[:], E[:], CBm[:].unsqueeze(1).to_broadcast([128, 8, 128]), ALU.mult, [E, CBm], [S])
                pc1 = k.ps()
                k.mm(pc1[:, 0:8], C[32:40, :], ident[32:40, 32:40], [C, ident], [pc1])
                k.act(ecum[:], pc1[:, 0:8], AF.Exp, [pc1], [ecum])
                k.ts(dg[32:40, :], ident[32:40, 32:40], C[32:40, last:last + 1], None, ALU.mult, None, [ident, C], [dg])
                pc2 = k.ps()
                k.mm(pc2[:, 0:8], L[32:40, :], dg[32:40, :], [L, dg], [pc2])
                k.act(dc[:], pc2[:, 0:8], AF.Exp, [pc2], [dc])
                k.tt(xw[:].rearrange("p (h q) -> p h q", h=8), xtok[:].rearrange("p (h q) -> p h q", h=8),
                     E[:, :, last:last + 1].to_broadcast([128, 8, 64]), ALU.mult, [xtok, E], [xw])
                pss = k.ps()
                k.mm(pss[:, :], btok[:], xw[:], [btok, xw], [pss])
                pyd = k.ps()
                for h in range(8):
                    k.mm(pyd[:, h * 64:(h + 1) * 64], S[:, h, :], xtok[:, h * 64:(h + 1) * 64], [S, xtok], [pyd])
                pyo = k.ps()
                k.mm(pyo[:, :], X[:, 5, :], Hst[:], [X, Hst], [pyo])
                k.tt(yo[:].rearrange("p (h q) -> p h q", h=8), pyo[:, :].rearrange("p (h q) -> p h q", h=8),
                     ecum[:].unsqueeze(2).to_broadcast([128, 8, 64]), ALU.mult, [pyo, ecum], [yo])
                k.tt(Y[:], yo[:], pyd[:, :], ALU.add, [yo, pyd], [Y])
                k.tt(Hst[:].rearrange("p (h q) -> p h q", h=8), Hst[:].rearrange("p (h q) -> p h q", h=8),
                     dc[:].unsqueeze(2).to_broadcast([128, 8, 64]), ALU.mult, [Hst, dc], [Hst])
                k.tt(Hst[:], Hst[:], pss[:, :], ALU.add, [Hst, pss], [Hst])
                if d == 0:
                    k.dma(yf_s[t0:t0 + 128, :], Y[:], r=[Y], q="pool")
                else:
                    k.dma(yfl[:], yf_s[t0:t0 + 128, :], w=[yfl], q="sp")
                    k.dma(zT[:], pT[CHI["z0"] * 128:(CHI["z0"] + 4) * 128, t0:t0 + 128].rearrange("(b p) t -> p b t", p=128), w=[zT], q="sp")
                    k.tt(Y[:], Y[:], yfl[:], ALU.add, [Y, yfl], [Y])
                    k.tt(yo[:].rearrange("p (h q) -> p h q", h=8), xtok[:].rearrange("p (h q) -> p h q", h=8),
                         dsk[:].unsqueeze(2).to_broadcast([128, 8, 64]), ALU.mult, [xtok, dsk], [yo])
                    k.tt(Y[:], Y[:], yo[:], ALU.add, [Y, yo], [Y])
                    k.act(zT[:], zT[:], AF.Silu, [zT], [zT])
                    pz = k.ps()
                    for b in range(4):
                        k.tr(pz[:, b * 128:(b + 1) * 128], zT[:, b, :], ident[:, :], [zT, ident], [pz])
                    k.tt(yg[:], Y[:], pz[:, :], ALU.mult, [Y, pz], [yg])
                    k.tt(junk[:], yg[:], yg[:], ALU.mult, [yg], [junk])
                    k.I("dve", "tensor_reduce", [junk], [ss], out=ss[:], in_=junk[:], axis=AX.X, op=ALU.add)
                    k.act(ss[:], ss[:], AF.Sqrt, [ss], [ss], scale=1.0 / 512, bias=k.eps_t[:, 0:1])
                    k.I("dve", "reciprocal", [ss], [ss], out=ss[:], in_=ss[:])
                    k.stt(yg[:], yg[:], ss[:, 0:1], nw[:], ALU.mult, ALU.mult, [yg, ss, nw], [yg])
                    po = k.ps()
                    for b in range(4):
                        k.tr(po[:, b * 128:(b + 1) * 128], yg[:, b * 128:(b + 1) * 128], ident[:, :], [yg, ident], [po])
                    k.copy(yot[:].rearrange("p b t -> p (b t)"), po[:, :], [po], [yot], eng="act")
                    k.dma(yT[2, :, t0:t0 + 128].rearrange("(b p) t -> p b t", p=128), yot[:], r=[yot], q="pool")
        k.barrier()


RCH = ["r0", "r1", "r2", "r3", "k0", "k1", "k2", "k3", "v0", "v1", "v2", "v3", "wlo0", "wlo1", "alo0", "alo1", "glo0", "glo1"]


def rwkv_phase(k, TC, TL, pT, yT, prm, ident, ones, one_t):
    T = TC + TL
    NCHK = T // 128
    rw_s = k.dram("rw_s", [2, 4, 4, 128, T], F32)
    vs_s = k.dram("vs_s", [4, 128, T], F32)
    pp_s = k.dram("pp_s", [2, 4, 128, T], F32)
    yr_s = k.dram("yr_s", [2, 4, T, 128], F32)
    gend = k.sb([128, 2, 4, NCHK], F32, "gend")
    gend_s = k.dram("gend_s", [128, 2 * 4 * NCHK], F32)
    gend2 = k.sb([64, 2, 2 * 4 * NCHK], F32, "gend2")
    blk1 = k.sb([128, 128], F32, "blk1")
    k.memset(blk1, 0.0)
    k.I("pool", "memset", [], [blk1], blk1[0:64, 0:64], 1.0)
    k.I("pool", "memset", [], [blk1], blk1[64:128, 64:128], 1.0)
    with contextlib.ExitStack() as st:
        PW = 512
        mu = k.sb([128, 18], F32, "mu", st); om = k.sb([128, 18], F32, "om", st); hm = k.sb([128, 18], F32, "hm", st)
        k.dma(mu[:], prm["rw_mu"], w=[mu])
        k.ts(om[:], mu[:], -1.0, 1.0, ALU.mult, ALU.add, [mu], [om])
        k.ts(hm[:], mu[:], 0.5, None, ALU.mult, None, [mu], [hm])
        w0 = k.sb([128, 2, 4], F32, "w0", st); a0 = k.sb([128, 2, 4], F32, "a0", st)
        kk_ = k.sb([128, 4], F32, "kk_", st); ka = k.sb([128, 4], F32, "ka", st); omka = k.sb([128, 4], F32, "omka", st); rk = k.sb([128, 4], F32, "rk", st)
        for t_, n_ in ((w0, "rw_w0"), (a0, "rw_a0"), (kk_, "rw_kk"), (ka, "rw_ka"), (rk, "rw_rk")):
            k.dma(t_[:], prm[n_], w=[t_])
        k.ts(omka[:], ka[:], -1.0, 1.0, ALU.mult, ALU.add, [ka], [omka])
        wup = k.sb([96, 2, 512], F32, "wup", st); aup = k.sb([96, 2, 512], F32, "aup", st); gup = k.sb([128, 2, 512], F32, "gup", st)
        k.dma(wup[:], prm["rw_wup"].rearrange("d k n -> k d n"), w=[wup]); k.dma(aup[:], prm["rw_aup"].rearrange("d k n -> k d n"), w=[aup])
        k.dma(gup[:], prm["rw_gup"].rearrange("(c p) n -> p c n", p=128), w=[gup])
        e12 = k.sb([128, 1], F32, "e12", st); k.memset(e12, 1e-12)
        mf = k.sb([128, PW], F32, "mf", st); mb = k.sb([128, PW], F32, "mb", st)
        k.memset(mf, 1.0); k.memset(mb, 1.0)
        k.I("pool", "memset", [], [mf], mf[:, 0::128], 0.0)
        k.I("pool", "memset", [], [mb], mb[:, 127::128], 0.0)
        Pin = [k.sb([128, PW + 2], F32, "Pin", st) for _ in range(3)]
        SH = [k.sb([128, PW], F32, "SH", st) for _ in range(18)]
        tmp = k.sb([128, PW], F32, "tmp", st)
        W = lambda nm: k.sb([128, PW], F32, nm, st)
        gT = W("gT"); kkn = W("kkn"); t1 = W("t1"); t2 = W("t2"); lw = W("lw"); lg = W("lg"); alr = W("alr"); key = W("key"); ks = W("ks")
        eneg = W("eneg"); epos = W("epos"); eex = W("eex")
        outs = [[W("o%d%d" % (i, j)) for j in range(4)] for i in range(2)]
        bon = W("bon")
        oi = 0
        for (s, n, isc) in segs_tiles(TC, TL, PW):
            s0, s1 = (0, TC) if isc else (TC, T)
            for ci, cn in enumerate(RCH):
                ch = CHI[cn]; m = CH[ch][1]
                P = Pin[ci % 3]
                lo = max(s0, s - 1); hi = min(s1, s + n + 1)
                k.dma(P[0:m, lo - (s - 1):hi - (s - 1)], pT[ch * 128:ch * 128 + m, lo:hi], w=[P], q="sp" if ci % 2 else "pool")
                if s - 1 < s0:
                    k.I("dve", "memset", [], [P], P[0:m, 0:1], 0.0)
                if s + n + 1 > s1:
                    k.I("dve", "memset", [], [P], P[0:m, n + 1:n + 2], 0.0)
                k.tt(tmp[0:m, 0:n], P[0:m, 0:n], P[0:m, 2:n + 2], ALU.add, [P], [tmp])
                k.ts(SH[ci][0:m, 0:n], P[0:m, 1:n + 1], om[0:m, ci:ci + 1], None, ALU.mult, None, [P, om], [SH[ci]])
                k.stt(SH[ci][0:m, 0:n], tmp[0:m, 0:n], hm[0:m, ci:ci + 1], SH[ci][0:m, 0:n], ALU.mult, ALU.add, [tmp, hm, SH[ci]], [SH[ci]])
            for ci in (12, 13):
                k.act(SH[ci][0:96, 0:n], SH[ci][0:96, 0:n], AF.Tanh, [SH[ci]], [SH[ci]])
            for ci in (16, 17):
                k.act(SH[ci][:, 0:n], SH[ci][:, 0:n], AF.Sigmoid, [SH[ci]], [SH[ci]])
            for hb in range(4):
                R_, K_, V_ = SH[hb], SH[4 + hb], SH[8 + hb]
                cs_ = slice(hb * 128, (hb + 1) * 128)
                ps = k.ps()
                for kc in range(2):
                    k.mm(ps[:, 0:n], gup[:, kc, cs_], SH[16 + kc][:, 0:n], [gup, SH[16 + kc]], [ps], start=(kc == 0), stop=(kc == 1))
                k.copy(gT[:, 0:n], ps[:, 0:n], [ps], [gT], eng="act")
                k.dma(pp_s[0, hb, :, s:s + n], gT[:, 0:n], r=[gT], q="pool")
                k.dma(vs_s[hb, :, s:s + n], V_[:, 0:n], r=[V_], q="pool")
                k.ts(t1[:, 0:n], K_[:, 0:n], kk_[:, hb:hb + 1], None, ALU.mult, None, [K_, kk_], [t1])
                k.tt(t2[:, 0:n], t1[:, 0:n], t1[:, 0:n], ALU.mult, [t1], [t2])
                ps = k.ps()
                k.mm(ps[:, 0:n], blk1[:], t2[:, 0:n], [blk1, t2], [ps])
                k.act(t2[:, 0:n], ps[:, 0:n], AF.Sqrt, [ps, e12], [t2], bias=e12[:, 0:1])
                k.I("dve", "reciprocal", [t2], [t2], out=t2[:, 0:n], in_=t2[:, 0:n])
                k.tt(kkn[:, 0:n], t1[:, 0:n], t2[:, 0:n], ALU.mult, [t1, t2], [kkn])
                for d in range(2):
                    ps = k.ps()
                    k.mm(ps[:, 0:n], wup[0:96, d, cs_], SH[12 + d][0:96, 0:n], [wup, SH[12 + d]], [ps])
                    k.act(lw[:, 0:n], ps[:, 0:n], AF.Sigmoid, [ps, w0], [lw], bias=w0[:, d, hb:hb + 1])
                    k.ts(lw[:, 0:n], lw[:, 0:n], -0.6065306597126334, None, ALU.mult, None, [lw], [lw])
                    if d == 0:
                        k.I("dve", "tensor_tensor_scan", [mf, lw], [lg], out=lg[:, 0:n], data0=mf[:, 0:n], data1=lw[:, 0:n], initial=0.0, op0=ALU.mult, op1=ALU.add)
                    else:
                        k.I("dve", "tensor_tensor_scan", [mb, lw], [lg], out=lg[:, n - 1::-1], data0=mb[:, n - 1::-1], data1=lw[:, n - 1::-1], initial=0.0, op0=ALU.mult, op1=ALU.add)
                    ps = k.ps()
                    k.mm(ps[:, 0:n], aup[0:96, d, cs_], SH[14 + d][0:96, 0:n], [aup, SH[14 + d]], [ps])
                    k.act(alr[:, 0:n], ps[:, 0:n], AF.Sigmoid, [ps, a0], [alr], bias=a0[:, d, hb:hb + 1])
                    k.ts(t1[:, 0:n], alr[:, 0:n], ka[:, hb:hb + 1], omka[:, hb:hb + 1], ALU.mult, ALU.add, [alr, ka, omka], [t1])
                    k.tt(key[:, 0:n], t1[:, 0:n], K_[:, 0:n], ALU.mult, [t1, K_], [key])
                    if d == 0:
                        k.copy(ks[:, 0:n], key[:, 0:n], [key], [ks], eng="pool")
                    else:
                        k.tt(ks[:, 0:n], ks[:, 0:n], key[:, 0:n], ALU.add, [ks, key], [ks], eng="pool")
                    k.act(eneg[:, 0:n], lg[:, 0:n], AF.Exp, [lg], [eneg], scale=-1.0)
                    k.act(epos[:, 0:n], lg[:, 0:n], AF.Exp, [lg], [epos])
                    k.tt(t2[:, 0:n], lg[:, 0:n], lw[:, 0:n], ALU.subtract, [lg, lw], [t2])
                    k.act(eex[:, 0:n], t2[:, 0:n], AF.Exp, [t2], [eex])
                    O = outs[oi % 2]; oi += 1
                    k.stt(O[0][:, 0:n], kkn[:, 0:n], -1.0, eex[:, 0:n], ALU.mult, ALU.mult, [kkn, eex], [O[0]])
                    k.tt(O[1][:, 0:n], R_[:, 0:n], epos[:, 0:n], ALU.mult, [R_, epos], [O[1]])
                    k.tt(t1[:, 0:n], kkn[:, 0:n], alr[:, 0:n], ALU.mult, [kkn, alr], [t1])
                    k.tt(O[2][:, 0:n], t1[:, 0:n], eneg[:, 0:n], ALU.mult, [t1, eneg], [O[2]])
                    k.tt(O[3][:, 0:n], key[:, 0:n], eneg[:, 0:n], ALU.mult, [key, eneg], [O[3]])
                    for kind in range(4):
                        k.dma(rw_s[d, hb, kind, :, s:s + n], O[kind][:, 0:n], r=[O[kind]], q=("sp", "pool")[kind % 2])
                    c0 = s // 128; ncs = n // 128
                    src = epos[:, 127:n:128] if d == 0 else epos[:, 0:n:128]
                    k.copy(gend[:, d, hb, c0:c0 + ncs], src, [epos], [gend], eng="pool")
                k.stt(t1[:, 0:n], R_[:, 0:n], rk[:, hb:hb + 1], ks[:, 0:n], ALU.mult, ALU.mult, [R_, rk, ks], [t1])
                ps = k.ps()
                k.mm(ps[:, 0:n], blk1[:], t1[:, 0:n], [blk1, t1], [ps])
                k.tt(bon[:, 0:n], ps[:, 0:n], V_[:, 0:n], ALU.mult, [ps, V_], [bon])
                k.dma(pp_s[1, hb, :, s:s + n], bon[:, 0:n], r=[bon], q="sp")
        k.dma(gend_s[:, :], gend[:].rearrange("p d b c -> p (d b c)"), r=[gend])
        k.barrier()
    k.dma(gend2[:], gend_s.rearrange("(h p) x -> p h x", h=2), w=[gend2])
    RW = "123"
    if "2" in RW:
      with contextlib.ExitStack() as st:
          def mk(name, pattern, cm, op):
              m_ = k.sb([128, 128], F32, name, st)
              k.memset(m_, 1.0)
              k.I("pool", "affine_select", [m_], [m_], out=m_[:], in_=m_[:], pattern=pattern, compare_op=op, fill=0.0, base=0, channel_multiplier=cm)
              return m_
          up_s = mk("up_s", [[1, 128]], -1, ALU.is_gt)
          up_i = mk("up_i", [[1, 128]], -1, ALU.is_ge)
          lo_s = mk("lo_s", [[-1, 128]], 1, ALU.is_gt)
          lo_i = mk("lo_i", [[-1, 128]], 1, ALU.is_ge)
          M4 = []
          for d in range(2):
              m4 = k.sb([128, 4, 128], F32, "m4_%d" % d, st)
              ms, mi = (up_s, up_i) if d == 0 else (lo_s, lo_i)
              for j, mm_ in enumerate((ms, mi, ms, mi)):
                  k.copy(m4[:, j, :], mm_[:], [mm_], [m4], eng="pool")
              M4.append(m4)
          NM = [lo_s, up_s]
          qps = []
          for j in range(4):
              for t_ in k.ps_tiles[2:]:
                  qps.append(Sub(t_, t_.t[:, j * 128:(j + 1) * 128], t_.name + "_q%d" % j))
          fps = k.ps_tiles[0:2]
          qi = [0]; fi = [0]

          def q():
              qi[0] += 1
              return qps[qi[0] % len(qps)]

          def fp():
              fi[0] += 1
              return fps[fi[0] % 2]
          chains = [(hb, d) for hb in range(4) for d in range(2)]
          cchunks = list(range(0, TC, 128)); lchunks = list(range(TC, T, 128))
          order = {0: cchunks + lchunks, 1: cchunks[::-1] + lchunks[::-1]}
          nsteps = len(order[0])
          GRP = 4
          STOP = 9
          def alloc_chain(st2):
              b = {}
              b["AR"] = [k.sb([128, 2, 2, 128], F32, "AR", st2) for _ in range(2)]
              for t_ in b["AR"]:
                  k.memset(t_, 0.0)
              b["BK2"] = [k.sb([64, 2, 2, 128], F32, "BK2", st2) for _ in range(2)]
              b["BKV"] = [k.sb([128, 3, 128], F32, "BKV", st2) for _ in range(2)]
              b["tok"] = k.sb([128, 3, 128], F32, "tok", st2)
              b["M4"] = [k.sb([128, 4, 128], F32, "M4h", st2) for _ in range(2)]
              b["N"] = [[k.sb([128, 128], F32, "N", st2) for _ in range(2)] for _ in range(2)]
              b["NT"] = [[k.sb([128, 128], F32, "NT", st2) for _ in range(2)] for _ in range(2)]
              b["X"] = [[k.sb([128, 64], F32, "X", st2) for _ in range(2)] for _ in range(2)]
              b["Y"] = k.sb([128, 128], F32, "Ych", st2)
              b["H"] = k.sb([128, 2, 64], F32, "Hst", st2)
              k.memset(b["H"], 0.0, eng="dve")
              return b
          for g0 in range(0, len(chains), GRP):
              grp = chains[g0:g0 + GRP]
              st2 = contextlib.ExitStack()
              B = {c: alloc_chain(st2) for c in grp}
              for step in range(nsteps):
                  U = {}
                  for c in grp:
                      hb, d = c; b = B[c]; t0 = order[d][step]
                      AR = b["AR"][step % 2]; BKV = b["BKV"][step % 2]
                      BK2 = b["BK2"][step % 2]
                      for h in range(2):
                          k.dma(AR[0:64, h, :, :], rw_s[d, hb, 0:2, h * 64:(h + 1) * 64, t0:t0 + 128].rearrange("k p t -> p k t"), w=[AR], q="sp")
                          k.dma(BK2[:, h, :, :], rw_s[d, hb, 2:4, h * 64:(h + 1) * 64, t0:t0 + 128].rearrange("k p t -> p k t"), w=[BK2], q="pool")
                      k.dma(BKV[:, 0:2, :], rw_s[d, hb, 2:4, :, t0:t0 + 128].rearrange("k p t -> p k t"), w=[BKV], q="pool")
                      k.dma(BKV[:, 2, :], vs_s[hb, :, t0:t0 + 128], w=[BKV], q="sp")
                  if STOP < 1: continue
                  for c in grp:
                      hb, d = c; b = B[c]
                      BKV = b["BKV"][step % 2]
                      pt = fp()
                      for j in range(3):
                          k.tr(pt[:, j * 128:(j + 1) * 128], BKV[:, j, :], ident[:, :], [BKV, ident], [pt])
                      k.copy(b["tok"][:].rearrange("p a b -> p (a b)"), pt[:, 0:384], [pt], [b["tok"]], eng="act")
                  if STOP < 2: continue
                  for c in grp:
                      hb, d = c; b = B[c]
                      AR = b["AR"][step % 2]; BK2 = b["BK2"][step % 2]
                      for h in range(2):
                          hr = slice(h * 64, (h + 1) * 64)
                          pf = fp()
                          k.mm(pf[:, 0:256], BK2[:, h, 0, :], AR[0:64, h, :, :], [BK2, AR], [pf])
                          k.mm(pf[:, 256:512], BK2[:, h, 1, :], AR[0:64, h, :, :], [BK2, AR], [pf])
                          k.tt(b["M4"][h][:].rearrange("p a b -> p (a b)"), pf[:, :], M4[d][:].rearrange("p a b -> p (a b)"), ALU.mult, [pf, M4[d]], [b["M4"][h]])
                          pq = q()
                          k.mm(pq[:, :], AR[0:64, h, 0, :], BK2[:, h, 0, :], [AR, BK2], [pq])
                          k.tt(b["N"][h][0][:], pq[:, :], NM[d][:], ALU.mult, [pq, NM[d]], [b["N"][h][0]])
                  if STOP < 3: continue
                  for c in grp:
                      hb, d = c; b = B[c]
                      AR = b["AR"][step % 2]
                      for h in range(2):
                          hr = slice(h * 64, (h + 1) * 64)
                          pq = q()
                          k.mm(pq[:, 0:64], AR[:, h, 0, :], b["H"][:, h, :], [AR, b["H"]], [pq], start=True, stop=False)
                          k.mm(pq[:, 0:64], b["M4"][h][:, 2, :], b["tok"][:, 2, hr], [b["M4"][h], b["tok"]], [pq], start=False, stop=True)
                          k.copy(b["X"][h][0][:], pq[:, 0:64], [pq], [b["X"][h][0]], eng="act")
                  if STOP < 4: continue
                  for lv in range(7):
                      for c in grp:
                          hb, d = c; b = B[c]
                          for h in range(2):
                              NTc = b["M4"][h][:, 0, :] if lv == 0 else b["NT"][h][lv % 2][:]
                              NTt = b["M4"][h] if lv == 0 else b["NT"][h][lv % 2]
                              Nc = b["N"][h][lv % 2]
                              Xc = b["X"][h][lv % 2]; Xn = b["X"][h][(lv + 1) % 2]
                              pq = q()
                              k.mm(pq[:, 0:64], NTc, Xc[:], [NTt, Xc], [pq])
                              k.tt(Xn[:], Xc[:], pq[:, 0:64], ALU.add, [Xc, pq], [Xn])
                              if lv < 6:
                                  Nn = b["N"][h][(lv + 1) % 2]; NTn = b["NT"][h][(lv + 1) % 2]
                                  p1 = q()
                                  k.mm(p1[:, :], NTc, Nc[:], [NTt, Nc], [p1])
                                  k.copy(Nn[:], p1[:, :], [p1], [Nn], eng="act")
                                  p2 = q()
                                  k.mm(p2[:, :], Nc[:], NTc, [Nc, NTt], [p2])
                                  k.copy(NTn[:], p2[:, :], [p2], [NTn], eng=("dve", "act")[h])
                  if STOP < 5: continue
                  for c in grp:
                      hb, d = c; b = B[c]; t0 = order[d][step]
                      AR = b["AR"][step % 2]
                      py = q(); phs = q()
                      for h in range(2):
                          hr = slice(h * 64, (h + 1) * 64)
                          Uh = b["X"][h][1]
                          k.mm(py[:, hr], AR[:, h, 1, :], b["H"][:, h, :], [AR, b["H"]], [py], start=True, stop=False)
                          k.mm(py[:, hr], b["M4"][h][:, 1, :], Uh[:], [b["M4"][h], Uh], [py], start=False, stop=False)
                          k.mm(py[:, hr], b["M4"][h][:, 3, :], b["tok"][:, 2, hr], [b["M4"][h], b["tok"]], [py], start=False, stop=True)
                          k.mm(phs[0:64, hr], b["tok"][:, 0, hr], Uh[:], [b["tok"], Uh], [phs], start=True, stop=False)
                          k.mm(phs[0:64, hr], b["tok"][:, 1, hr], b["tok"][:, 2, hr], [b["tok"]], [phs], start=False, stop=True)
                      k.copy(b["Y"][:], py[:, :], [py], [b["Y"]], eng="act")
                      k.dma(yr_s[d, hb, t0:t0 + 128, :], b["Y"][:], r=[b["Y"]], q="pool")
                      k.tt(b["H"][0:64], b["H"][0:64], phs[0:64, :].rearrange("p (h v) -> p h v", h=2), ALU.add, [b["H"], phs], [b["H"]])
                      gi = (d * 4 + hb) * NCHK + t0 // 128
                      k.tt(b["H"][0:64], b["H"][0:64], gend2[:, :, gi:gi + 1].to_broadcast([64, 2, 64]), ALU.mult, [b["H"], gend2], [b["H"]])
              k.barrier()
              st2.close()
          k.barrier()
    if "3" in RW:
      with contextlib.ExitStack() as st:
          lnw = k.sb([128, 4], F32, "lnw", st); lnb = k.sb([128, 4], F32, "lnb", st)
          k.dma(lnw[:], prm["rw_lnw"], w=[lnw]); k.dma(lnb[:], prm["rw_lnb"], w=[lnb])
          egn = k.sb([128, 1], F32, "egn", st); k.memset(egn, 64e-5)
          NB_ = 4
          Y0 = [k.sb([128, NB_, 128], F32, "Y0", st) for _ in range(2)]; Y1 = [k.sb([128, NB_, 128], F32, "Y1", st) for _ in range(2)]
          GB = [k.sb([128, 2, NB_ * 128], F32, "GB", st) for _ in range(2)]
          s1 = k.sb([128, NB_ * 2], F32, "s1", st); s2 = k.sb([128, NB_ * 2], F32, "s2", st)
          yc = k.sb([128, NB_, 128], F32, "yc", st); sq = k.sb([128, NB_, 128], F32, "sq", st)
          OT = [k.sb([128, NB_ * 128], F32, "OT", st) for _ in range(2)]
          it = 0
          for hb in range(4):
              for t0 in range(0, T, NB_ * 128):
                  nb = min(NB_, (T - t0) // 128); n = nb * 128
                  a, b_, gb, ot = Y0[it % 2], Y1[it % 2], GB[it % 2], OT[it % 2]; it += 1
                  k.dma(a[:, 0:nb, :], yr_s[0, hb, t0:t0 + n, :].rearrange("(c p) v -> p c v", p=128), w=[a], q="sp")
                  k.dma(b_[:, 0:nb, :], yr_s[1, hb, t0:t0 + n, :].rearrange("(c p) v -> p c v", p=128), w=[b_], q="pool")
                  k.dma(gb[:, :, 0:n], pp_s[:, hb, :, t0:t0 + n].rearrange("k p t -> p k t"), w=[gb], q="sp")
                  k.tt(a[:, 0:nb, :], a[:, 0:nb, :], b_[:, 0:nb, :], ALU.add, [a, b_], [a])
                  v4 = lambda t_: t_[:, 0:nb, :].rearrange("p c (h v) -> p (c h) v", h=2)
                  k.I("dve", "tensor_reduce", [a], [s1], out=s1[:, 0:nb * 2], in_=v4(a), axis=AX.X, op=ALU.add)
                  k.ts(s1[:, 0:nb * 2], s1[:, 0:nb * 2], 1.0 / 64, None, ALU.mult, None, [s1], [s1])
                  k.tt(v4(yc), v4(a), s1[:, 0:nb * 2].unsqueeze(2).to_broadcast([128, nb * 2, 64]), ALU.subtract, [a, s1], [yc])
                  k.tt(sq[:, 0:nb, :], yc[:, 0:nb, :], yc[:, 0:nb, :], ALU.mult, [yc], [sq])
                  k.I("dve", "tensor_reduce", [sq], [s2], out=s2[:, 0:nb * 2], in_=v4(sq), axis=AX.X, op=ALU.add)
                  k.act(s2[:, 0:nb * 2], s2[:, 0:nb * 2], AF.Sqrt, [s2, egn], [s2], scale=1.0 / 64, bias=egn[:, 0:1])
                  k.I("dve", "reciprocal", [s2], [s2], out=s2[:, 0:nb * 2], in_=s2[:, 0:nb * 2])
                  k.tt(v4(yc), v4(yc), s2[:, 0:nb * 2].unsqueeze(2).to_broadcast([128, nb * 2, 64]), ALU.mult, [yc, s2], [yc])
                  pt = k.ps_tiles[it % 2]
                  for c in range(nb):
                      k.tr(pt[:, c * 128:(c + 1) * 128], yc[:, c, :], ident[:, :], [yc, ident], [pt])
                  k.ts(ot[:, 0:n], pt[:, 0:n], lnw[:, hb:hb + 1], lnb[:, hb:hb + 1], ALU.mult, ALU.add, [pt, lnw, lnb], [ot])
                  k.tt(ot[:, 0:n], ot[:, 0:n], gb[:, 1, 0:n], ALU.add, [ot, gb], [ot])
                  k.tt(ot[:, 0:n], ot[:, 0:n], gb[:, 0, 0:n], ALU.mult, [ot, gb], [ot])
                  k.dma(yT[1, hb * 128:(hb + 1) * 128, t0:t0 + n], ot[:, 0:n], r=[ot], q="pool")
          k.barrier()


CH = ([("lx%d" % i, 128) for i in range(4)] + [("lg%d" % i, 128) for i in range(4)] +
      [("r%d" % i, 128) for i in range(4)] + [("k%d" % i, 128) for i in range(4)] + [("v%d" % i, 128) for i in range(4)] +
      [("wlo0", 96), ("wlo1", 96), ("alo0", 96), ("alo1", 96), ("glo0", 128), ("glo1", 128)] +
      [("z%d" % i, 128) for i in range(4)] + [("sx%d" % i, 128) for i in range(4)] + [("sB", 128), ("sC", 128), ("dt0", 8), ("dt1", 8)])
CHI = {n: i for i, (n, w) in enumerate(CH)}
CHOFF = np.cumsum([0] + [w for n, w in CH]).tolist()
NCOL = CHOFF[-1]
NCH = len(CH)


def segs_tiles(TC, TL, n):
    out = [(s, min(n, TC - s), 1) for s in range(0, TC, n)]
    out += [(TC + s, min(n, TL - s), 0) for s in range(0, TL, n)]
    return out


def conv_seg(k, out, p, cw, cb, segs, eng="dve"):
    w = lambda i: cw[1][:, cw[2] + i:cw[2] + i + 1]
    for (s, e) in segs:
        k.ts(out[:, s:e], p[:, s:e], w(2), cb[1][:, cb[2]:cb[2] + 1], ALU.mult, ALU.add, [p, cw[0], cb[0]], [out])
        k.stt(out[:, s + 2:e], p[:, s:e - 2], w(0), out[:, s + 2:e], ALU.mult, ALU.add, [p, cw[0], out], [out])
        k.stt(out[:, s + 1:e], p[:, s:e - 1], w(1), out[:, s + 1:e], ALU.mult, ALU.add, [p, cw[0], out], [out])
        k.stt(out[:, s:e - 1], p[:, s + 1:e], w(3), out[:, s:e - 1], ALU.mult, ALU.add, [p, cw[0], out], [out])


def build_A(TC, TL, do=("lru", "ssd", "rwkv"), dbg_p=False):
    T = TC + TL
    nc = bass.Bass("TRN2", target_bir_lowering=False)
    k = KB(nc)
    inp = lambda n, s: k.dram(n, s, F32, "ExternalInput")
    hT = inp("hT", [D, T]); cT = inp("cT", [D, 2]); ada_w = inp("ada_w", [D, 2 * D]); ada_b_l = inp("ada_b_l", [128, 32])
    nmix = inp("nmix", [128, KC]); w_sel = inp("w_sel", [D, NCOL])
    lru_cw = inp("lru_cw", [128, 4, 4]); lru_cb = inp("lru_cb", [128, 4]); lru_gw = inp("lru_gw", [2, 2, 4, 128, 128])
    lru_gb = inp("lru_gb", [128, 2, 2, 4]); lru_lam = inp("lru_lam", [128, 2, 4])
    yT = k.dram("yT", [3, 512, T], F32, "ExternalOutput")
    uT = k.dram("uT_s", [D, T], BF16)
    pT = k.dram("pT_s", [NCH * 128, T], F32, kind="ExternalOutput" if dbg_p else "Internal")
    k.init_psum()
    ones, ident = consts(k)
    one_t = k.sb([128, 1], F32, "one_t"); k.memset(one_t, 1.0)
    mod = modulation(k, cT, ada_w, ada_b_l, 32, 0)
    nm = k.sb([128, KC], F32, "nm"); k.dma(nm[:], nmix, w=[nm])
    A_mix = k.sb([128, KC, 2], F32, "A_mix")
    k.ts(A_mix[:], mod[:, KC:2 * KC, :], 1.0, None, ALU.add, None, [mod], [A_mix])
    k.tt(A_mix[:], A_mix[:], nm[:].unsqueeze(2).to_broadcast([128, KC, 2]), ALU.mult, [A_mix, nm], [A_mix])

    def tview(ap2d, t0, nt):
        return ap2d[:, t0:t0 + nt].rearrange("(c p) t -> p c t", p=128)

    NTA = 512
    with contextlib.ExitStack() as st:
        Hs = [k.sb([128, KC, NTA], F32, "H", st) for _ in range(2)]
        tmp32 = k.sb([128, KC, NTA], F32, "tmp32", st)
        Us = [k.sb([128, KC, NTA], BF16, "U", st) for _ in range(2)]
        ssum = k.sb([128, NTA], F32, "ssum", st); rstd = k.sb([128, NTA], F32, "rstd", st)
        for i, (t0, nt, j) in enumerate(segs_tiles(TC, TL, NTA)):
            H = Hs[i % 2]; U = Us[i % 2]
            k.dma(H[:, :, 0:nt], tview(hT, t0, nt), w=[H], q="sp")
            norm_mod(k, H, nt, ones, A_mix, mod, j, U, tmp32, ssum, rstd, sh_off=0)
            k.dma(tview(uT, t0, nt), U[:, :, 0:nt], r=[U], q="pool")
        k.barrier()
    with contextlib.ExitStack() as st:
        GW = 10
        groups = [list(range(g, min(g + GW, NCH))) for g in range(0, NCH, GW)]
        wg = [k.sb([128, KC, GW * 128], BF16, "wg", st) for _ in range(2)]
        stg = [k.sb([128, KC, 128], F32, "stg", st) for _ in range(3)]
        Us = [k.sb([128, KC, NTA], BF16, "U2", st) for _ in range(2)]
        ob = [k.sb([128, NTA], F32, "ob", st) for _ in range(4)]
        si = 0; ui = 0; oi = 0
        for gi, grp in enumerate(groups):
            W = wg[gi % 2]
            for ci, ch in enumerate(grp):
                m = CH[ch][1]
                s_ = stg[si % 3]; si += 1
                k.dma(s_[:, :, 0:m], wview(w_sel, 0, KC, CHOFF[ch], m), w=[s_], q="sp" if si % 2 else "pool")
                k.copy(W[:, :, ci * 128:ci * 128 + m], s_[:, :, 0:m], [s_], [W], eng=("dve", "pool")[si % 2])
            for (t0, nt, j) in segs_tiles(TC, TL, NTA):
                U = Us[ui % 2]; ui += 1
                k.dma(U[:, :, 0:nt], tview(uT, t0, nt), w=[U], q="sp")
                for ci, ch in enumerate(grp):
                    m = CH[ch][1]
                    ps = k.ps()
                    for c in range(KC):
                        k.mm(ps[0:m, 0:nt], W[:, c, ci * 128:ci * 128 + m], U[:, c, 0:nt], [W, U], [ps], start=(c == 0), stop=(c == KC - 1))
                    o = ob[oi % 4]; oi += 1
                    k.copy(o[0:m, 0:nt], ps[0:m, 0:nt], [ps], [o], eng=("act", "dve")[oi % 2])
                    k.dma(pT[ch * 128:ch * 128 + m, t0:t0 + nt], o[0:m, 0:nt], r=[o], q="pool")
        k.barrier()

    segs = [(0, TC), (TC, T)]
    TT = 1024
    if "lru" in do:
        with contextlib.ExitStack() as st:
            cw = k.sb([128, 4, 4], F32, "lcw", st); cb = k.sb([128, 4], F32, "lcb", st)
            gb = k.sb([128, 2, 2, 4], F32, "lgb", st); lam = k.sb([128, 2, 4], F32, "llam", st); cs = k.sb([128, 2, 4], F32, "lcs", st)
            cs2 = k.sb([128, 2, 4], F32, "lcs2", st)
            gw = k.sb([128, 2, 2, 4, 128], F32, "lgw", st)
            k.dma(cw[:], lru_cw, w=[cw]); k.dma(cb[:], lru_cb, w=[cb]); k.dma(gb[:], lru_gb, w=[gb]); k.dma(lam[:], lru_lam, w=[lam])
            k.dma(gw[:].rearrange("p d g n j -> p (d g n) j"), lru_gw.rearrange("d g n k j -> k (d g n) j"), w=[gw])
            k.act(cs[:], lam[:], AF.Exp, [lam], [cs], scale=-1.0)
            k.act(cs[:], cs[:], AF.Ln, [cs], [cs], bias=one_t[:, 0:1])
            k.ts(cs2[:], cs[:], -16.0, None, ALU.mult, None, [cs], [cs2])
            k.ts(cs[:], cs[:], -8.0, None, ALU.mult, None, [cs], [cs])
            xc = k.sb([128, T], F32, "xc", st); hs = k.sb([128, T], F32, "hs", st)
            rg = k.sb([128, TT], F32, "rg", st); ig = k.sb([128, TT], F32, "ig", st); a_t = k.sb([128, TT], F32, "a_t", st)
            bx = k.sb([128, TT], F32, "bx", st); hb = [k.sb([128, TT], F32, "hb", st) for _ in range(2)]
            tiles = segs_tiles(TC, TL, TT)
            ctx_tiles = [t for t in tiles if t[2] == 1]; lat_tiles = [t for t in tiles if t[2] == 0]
            for blk in range(4):
                k.dma(hs[:], pT[CHI["lx0"] * 128 + blk * 128: CHI["lx0"] * 128 + (blk + 1) * 128, :], w=[hs])
                conv_seg(k, xc, hs, (cw, cw[:, blk, :], 0), (cb, cb, blk), segs)
                for d in range(2):
                    order = (ctx_tiles + lat_tiles) if d == 0 else (ctx_tiles[::-1] + lat_tiles[::-1])
                    prev = None
                    for ti, (s, n, isc) in enumerate(order):
                        for g, dst in ((0, rg), (1, ig)):
                            for c0 in range(0, n, 512):
                                cn = min(512, n - c0)
                                ps = k.ps()
                                k.mm(ps[:, 0:cn], gw[:, d, g, blk, :], xc[:, s + c0:s + c0 + cn], [gw, xc], [ps])
                                k.act(dst[:, c0:c0 + cn], ps[:, 0:cn], AF.Sigmoid, [ps, gb], [dst], bias=gb[:, d, g, blk:blk + 1])
                        k.act(a_t[:, 0:n], rg[:, 0:n], AF.Exp, [rg, cs], [a_t], scale=cs[:, d, blk:blk + 1])
                        k.act(rg[:, 0:n], rg[:, 0:n], AF.Exp, [rg, cs2], [rg], scale=cs2[:, d, blk:blk + 1])
                        k.ts(rg[:, 0:n], rg[:, 0:n], -1.0, 1.0, ALU.mult, ALU.add, [rg], [rg])
                        k.act(rg[:, 0:n], rg[:, 0:n], AF.Sqrt, [rg], [rg])
                        k.tt(bx[:, 0:n], rg[:, 0:n], ig[:, 0:n], ALU.mult, [rg, ig], [bx])
                        k.tt(bx[:, 0:n], bx[:, 0:n], xc[:, s:s + n], ALU.mult, [bx, xc], [bx])
                        if d == 0:
                            init = 0.0 if prev is None else prev
                            k.I("dve", "tensor_tensor_scan", [a_t, bx, hs], [hs], out=hs[:, s:s + n], data0=a_t[:, 0:n], data1=bx[:, 0:n],
                                initial=init, op0=ALU.mult, op1=ALU.add)
                            prev = hs[:, s + n - 1:s + n]
                        else:
                            hbt = hb[ti % 2]
                            init = 0.0 if prev is None else prev[0][:, 0:1]
                            rd = [a_t, bx] + ([prev[1]] if prev is not None else [])
                            k.I("dve", "tensor_tensor_scan", rd, [hbt], out=hbt[:, n - 1::-1], data0=a_t[:, n - 1::-1], data1=bx[:, n - 1::-1],
                                initial=init, op0=ALU.mult, op1=ALU.add)
                            prev = (hbt, hbt)
                            k.tt(hs[:, s:s + n], hs[:, s:s + n], hbt[:, 0:n], ALU.add, [hs, hbt], [hs], eng="pool")
                for (s, n, isc) in tiles:
                    k.dma(rg[:, 0:n], pT[CHI["lg0"] * 128 + blk * 128:CHI["lg0"] * 128 + (blk + 1) * 128, s:s + n], w=[rg])
                    k.tt(ig[:, 0:n], rg[:, 0:n], rg[:, 0:n], ALU.mult, [rg], [ig])
                    k.ts(ig[:, 0:n], ig[:, 0:n], 0.044715, 1.0, ALU.mult, ALU.add, [ig], [ig])
                    k.tt(ig[:, 0:n], ig[:, 0:n], rg[:, 0:n], ALU.mult, [ig, rg], [ig])
                    k.act(ig[:, 0:n], ig[:, 0:n], AF.Sigmoid, [ig], [ig], scale=1.5957691216057308)
                    k.tt(ig[:, 0:n], ig[:, 0:n], rg[:, 0:n], ALU.mult, [ig, rg], [ig])
                    k.tt(bx[:, 0:n], ig[:, 0:n], hs[:, s:s + n], ALU.mult, [ig, hs], [bx])
                    k.dma(yT[0, blk * 128:(blk + 1) * 128, s:s + n], bx[:, 0:n], r=[bx])
            k.barrier()
    if "ssd" in do:
        prm = dict(ssm_cw=inp("ssm_cw", [128, 6, 4]), ssm_cb=inp("ssm_cb", [128, 6]), ssm_A=inp("ssm_A", [8, 2]), ssm_dtb=inp("ssm_dtb", [8, 2]),
                   ssm_dsk=inp("ssm_dsk", [1, 8]), ssm_nw=inp("ssm_nw", [1, 512]))
        ssd_phase(k, TC, TL, pT, yT, prm, ident, ones, one_t)
    if "rwkv" in do:
        prm = dict(rw_mu=inp("rw_mu", [128, 18]), rw_w0=inp("rw_w0", [128, 2, 4]), rw_a0=inp("rw_a0", [128, 2, 4]), rw_kk=inp("rw_kk", [128, 4]),
                   rw_ka=inp("rw_ka", [128, 4]), rw_rk=inp("rw_rk", [128, 4]), rw_wup=inp("rw_wup", [2, 96, 512]), rw_aup=inp("rw_aup", [2, 96, 512]),
                   rw_gup=inp("rw_gup", [256, 512]), rw_lnw=inp("rw_lnw", [128, 4]), rw_lnb=inp("rw_lnb", [128, 4]))
        rwkv_phase(k, TC, TL, pT, yT, prm, ident, ones, one_t)
    k.finish()
    return nc, k


from concourse.bass_utils import run_bass_kernel_spmd

SEQ_, CTX_, GRID_W_ = 8192, 256, 64
OFF_LRU_, OFF_RWKV_, OFF_SSM_ = 6144, 10240, 17024
D_XBC_ = 3072
_PROG = {}


def _col_sel(q):
    cols = []
    O = OFF_LRU_
    cols += list(range(O + 512 * q, O + 512 * q + 512)); cols += list(range(O + 2048 + 512 * q, O + 2048 + 512 * q + 512))
    O = OFF_RWKV_
    for j in range(3):
        cols += list(range(O + 2048 * j + 512 * q, O + 2048 * j + 512 * q + 512))
    cols += list(range(O + 6144, O + 6144 + 640))
    O = OFF_SSM_
    cols += list(range(O + 512 * q, O + 512 * q + 512)); cols += list(range(O + 2048 + 512 * q, O + 2048 + 512 * q + 512))
    cols += list(range(O + 4096 + 128 * q, O + 4096 + 128 * q + 128)); cols += list(range(O + 4096 + 512 + 128 * q, O + 4096 + 512 + 128 * q + 128))
    O2 = O + 2048 + D_XBC_
    cols += list(range(O2 + 8 * q, O2 + 8 * q + 8)); cols += list(range(O2 + 32 + 8 * q, O2 + 32 + 8 * q + 8))
    return np.array(cols)


def _fm(v):
    return np.ascontiguousarray(np.asarray(v, np.float32).reshape(-1, 128).T)


def _stageA_params(P, li, q):
    sl = slice(512 * q, 512 * q + 512)
    hs_ = slice(8 * q, 8 * q + 8)
    ca = np.ascontiguousarray
    d = dict(
        ada_w=ca(P["ada_w"][li][:, :4096]), ada_b_l=_fm(P["ada_b"][li][:4096]), nmix=_fm(P["norm_mix"][li]),
        w_sel=ca(P["w_in"][li][:, _col_sel(q)]),
        lru_cw=ca(P["lru_conv_w"][li][:, sl].reshape(4, 4, 128).transpose(2, 1, 0)),
        lru_cb=_fm(P["lru_conv_b"][li][sl]), lru_gw=ca(P["lru_gate_w"][li][:, :, 4 * q:4 * q + 4]),
        lru_gb=ca(P["lru_gate_b"][li][:, :, sl].reshape(2, 2, 4, 128).transpose(3, 0, 1, 2)),
        lru_lam=ca(P["lru_lambda"][li][:, sl].reshape(2, 4, 128).transpose(2, 0, 1)))
    xbc_cols = np.concatenate([np.arange(512 * q, 512 * q + 512), 2048 + np.arange(128 * q, 128 * q + 128),
                               2048 + 512 + np.arange(128 * q, 128 * q + 128)])
    d.update(ssm_cw=ca(P["ssm_conv_w"][li][:, xbc_cols].reshape(4, 6, 128).transpose(2, 1, 0)),
             ssm_cb=_fm(P["ssm_conv_b"][li][xbc_cols]), ssm_A=ca(P["ssm_a_log"][li][:, hs_].T),
             ssm_dtb=ca(P["ssm_dt_bias"][li][:, hs_].T), ssm_dsk=ca(P["ssm_d"][li][hs_][None]), ssm_nw=ca(P["ssm_norm_w"][li][sl][None]))
    mu = P["rwkv_mu"][li]
    pad = lambda v: np.concatenate([v, np.zeros(128 - len(v), np.float32)])
    mucols = [mu[j * 2048 + 512 * q + i * 128: j * 2048 + 512 * q + (i + 1) * 128] for j in range(3) for i in range(4)]
    mucols += [pad(mu[6144 + i * 96:6144 + (i + 1) * 96]) for i in range(4)] + [mu[6144 + 384:6144 + 512], mu[6144 + 512:6144 + 640]]
    f4 = lambda v: ca(v[sl].reshape(4, 128).T)
    d.update(rw_mu=ca(np.stack(mucols, 1)), rw_w0=ca(P["rwkv_w0"][li][:, sl].reshape(2, 4, 128).transpose(2, 0, 1)),
             rw_a0=ca(P["rwkv_a0"][li][:, sl].reshape(2, 4, 128).transpose(2, 0, 1)), rw_kk=f4(P["rwkv_k_k"][li]), rw_ka=f4(P["rwkv_k_a"][li]),
             rw_rk=f4(P["rwkv_r_k"][li].reshape(-1)), rw_wup=ca(P["rwkv_w_up"][li][:, :, sl]), rw_aup=ca(P["rwkv_a_up"][li][:, :, sl]),
             rw_gup=ca(P["rwkv_g_up"][li][:, sl]), rw_lnw=f4(P["rwkv_ln_w"][li]), rw_lnb=f4(P["rwkv_ln_b"][li]))
    return {k_: np.asarray(v, np.float32) for k_, v in d.items()}


def _perm(a, rows, cols):
    return a.reshape(rows, cols, *a.shape[1:]).swapaxes(0, 1).reshape(a.shape)


def kernel(**inputs):
    P = {k_: np.asarray(v, np.float32) for k_, v in inputs.items()}
    Bn, TL, Dm = P["x"].shape
    TC = P["ctx"].shape[1]
    T = TC + TL
    rows = TL // GRID_W_
    depth = P["w_in"].shape[0]
    h_lat = P["x"].copy(); h_ctx = P["ctx"].copy()
    ncore = 8
    QN = ncore // Bn
    NLs = TL // QN; NCs = TC // QN
    if "A" not in _PROG:
        _PROG["A"] = build_A(TC, TL)[0]
    for li in range(depth):
        last = li == depth - 1
        odd = li % 2 == 1
        in_maps = []
        for b in range(Bn):
            lat = _perm(h_lat[b], rows, GRID_W_) if odd else h_lat[b]
            hT = np.ascontiguousarray(np.concatenate([h_ctx[b], lat], 0).T)
            cT = np.ascontiguousarray(np.stack([P["c"][b], P["c_ctx"]], 1))
            for q in range(QN):
                m = _stageA_params(P, li, q)
                m.update(hT=hT, cT=cT)
                in_maps.append(m)
        res = run_bass_kernel_spmd(_PROG["A"], in_maps, core_ids=list(range(ncore)))
        y_lat = np.empty((Bn, 3, TL, Dm), np.float32); y_ctx = np.empty((Bn, 3, TC, Dm), np.float32)
        for b in range(Bn):
            for q in range(QN):
                yT = np.asarray(res.results[b * QN + q]["yT"])
                for br in range(3):
                    yy = yT[br].T
                    y_ctx[b, br, :, 512 * q:512 * q + 512] = yy[:TC]
                    latp = yy[TC:]
                    y_lat[b, br, :, 512 * q:512 * q + 512] = _perm(latp, GRID_W_, rows) if odd else latp
        del res
        ncx = 0 if last else NCs
        kind = "dense" if li % 2 == 0 else "moe"
        key = ("B", kind, ncx, last)
        if key not in _PROG:
            _PROG[key] = build_B(NLs, ncx, kind, last)
        j = li // 2
        common = dict(ada_w=P["ada_w"][li], ada_b_l=_fm(P["ada_b"][li]), nmix=_fm(P["norm_mix"][li]), nffn=_fm(P["norm_ffn"][li]),
                      nfin=_fm(P["norm_final"]), w_gate=np.ascontiguousarray(P["w_in"][li][:, :6144]),
                      w_outs=np.ascontiguousarray(np.stack([P["w_out_lru"][li], P["w_out_rwkv"][li], P["w_out_ssm"][li]], 0)), w_o=P["w_o"][li])
        if kind == "dense":
            common.update(w1=P["ffn_w1"][j], w3=P["ffn_w3"][j], w2=P["ffn_w2"][j])
        else:
            common.update(router=P["moe_router"][j], mw1=P["moe_w1"][j], mw3=P["moe_w3"][j], mw2=P["moe_w2"][j])
        in_maps = []
        for b in range(Bn):
            cT = np.ascontiguousarray(np.stack([P["c"][b], P["c_ctx"]], 1))
            for s in range(QN):
                ls = slice(NLs * s, NLs * (s + 1)); cs = slice(ncx * s, ncx * (s + 1))
                hh = np.concatenate([h_lat[b, ls], h_ctx[b, cs]], 0) if ncx else h_lat[b, ls]
                yy = np.concatenate([y_lat[b, :, ls], y_ctx[b, :, cs]], 1) if ncx else y_lat[b, :, ls]
                m = dict(common)
                m.update(hT=np.ascontiguousarray(hh.T), yT=np.ascontiguousarray(yy.transpose(0, 2, 1)), cT=cT)
                in_maps.append(m)
        res = run_bass_kernel_spmd(_PROG[key], in_maps, core_ids=list(range(ncore)))
        for b in range(Bn):
            for s in range(QN):
                o = np.asarray(res.results[b * QN + s]["outT"]).T
                h_lat[b, NLs * s:NLs * (s + 1)] = o[:NLs]
                if ncx:
                    h_ctx[b, ncx * s:ncx * (s + 1)] = o[NLs:]
        del res
    return h_lat
```
